# Optimizing a Trainium2 kernel written in Bass

```python
import jax, jax.numpy as jnp
from jax import lax
import numpy as np

D_MODEL = 1024
BATCH = 8
SEQ = 2048
DEPTH = 2

HEAD_DIM = 64
ROT_DIM = HEAD_DIM // 4
ROPE_THETA = 500000.0
EPS = 1e-6
NSA_HEADS = 8
NSA_KV_HEADS = 2
CMP_LEN = 32
CMP_STRIDE = 16
CMP_HIDDEN = 128
SEL_LEN = 64
N_SEL = 16
WIN = 512
SEL_Q_BLOCK = 64
DIL_PATTERNS = ((128, 1), (512, 4), (2048, 16))
N_DIL_GROUPS = 3
DIL_HEADS = 4
ATTN_BLOCK = 128
N_BRANCHES = 2
D_FF = 2816
N_EXPERTS = 8
TOP_K = 2
N_DENSE = (DEPTH + 1) // 2
N_MOE = DEPTH // 2

Q_A_COLS = NSA_HEADS * HEAD_DIM
KV_A_COLS = 3 * 2 * NSA_KV_HEADS * HEAD_DIM
GATE_A_COLS = NSA_HEADS * 3
QKV_B_COLS = 3 * N_DIL_GROUPS * DIL_HEADS * HEAD_DIM
MERGE_COLS = N_BRANCHES * D_MODEL
IN_COLS = Q_A_COLS + KV_A_COLS + GATE_A_COLS + QKV_B_COLS + MERGE_COLS
SPLITS = (Q_A_COLS, Q_A_COLS + KV_A_COLS, Q_A_COLS + KV_A_COLS + GATE_A_COLS,
          Q_A_COLS + KV_A_COLS + GATE_A_COLS + QKV_B_COLS)
OUT_A = NSA_HEADS * HEAD_DIM
OUT_B = DIL_HEADS * HEAD_DIM

kernel_name = "nsa_dilated_gated_hybrid_moe"

F32 = jnp.float32


def rms_norm(x, g):
    x32 = x.astype(F32)
    y = x32 * lax.rsqrt(jnp.mean(x32 * x32, axis=-1, keepdims=True) + EPS)
    return (y * g.astype(F32)).astype(x.dtype)


def rope_tables(positions):
    inv = ROPE_THETA ** (-jnp.arange(0, ROT_DIM, 2, dtype=F32) / ROT_DIM)
    ang = positions.astype(F32)[..., None] * inv
    return jnp.cos(ang)[:, :, None, :], jnp.sin(ang)[:, :, None, :]


def apply_rope(x, cos, sin):
    half = ROT_DIM // 2
    xf = x.astype(F32)
    x1, x2 = xf[..., :half], xf[..., half:ROT_DIM]
    out = jnp.concatenate([x1 * cos - x2 * sin, x2 * cos + x1 * sin, xf[..., ROT_DIM:]], axis=-1)
    return out.astype(x.dtype)


def masked_softmax(s, mask):
    s = jnp.where(mask, s, -jnp.inf)
    m = jnp.max(s, axis=-1, keepdims=True)
    m = jnp.where(jnp.isfinite(m), m, 0.0)
    e = jnp.where(mask, jnp.exp(s - m), 0.0)
    den = jnp.sum(e, axis=-1, keepdims=True)
    return e / jnp.where(den > 0, den, 1.0)


def banded_attention(q, k, v, n_back):
    B, L, H, dh = q.shape
    G = k.shape[2]
    hpg = H // G
    C = ATTN_BLOCK
    nb = -(-L // C)
    tail = nb * C - L
    npv = -(-n_back // C)
    q = jnp.pad(q, ((0, 0), (0, tail), (0, 0), (0, 0)))
    pad_kv = ((0, 0), (npv * C, tail), (0, 0), (0, 0))
    kb = jnp.pad(k, pad_kv).reshape(B, nb + npv, C, G, dh)
    vb = jnp.pad(v, pad_kv).reshape(B, nb + npv, C, G, dh)
    win = jnp.arange(nb)[:, None] + jnp.arange(npv + 1)[None, :]
    K = (npv + 1) * C
    kw = kb[:, win].reshape(B, nb, K, G, dh)
    vw = vb[:, win].reshape(B, nb, K, G, dh)
    qb = q.reshape(B, nb, C, G, hpg, dh)
    s = jnp.einsum('bnqghd,bnkgd->bnghqk', qb.astype(F32), kw.astype(F32)) * (dh ** -0.5)
    t = jnp.arange(nb)[:, None] * C + jnp.arange(C)[None, :]
    kpos = (jnp.arange(nb)[:, None] - npv) * C + jnp.arange(K)[None, :]
    diff = t[:, :, None] - kpos[:, None, :]
    mask = (diff >= 0) & (diff <= n_back) & (kpos[:, None, :] >= 0)
    s = jnp.where(mask[None, :, None, None], s, -jnp.inf)
    lse = jax.nn.logsumexp(s, axis=-1)
    p = jnp.exp(s - lse[..., None])
    o = jnp.einsum('bnghqk,bnkgd->bnqghd', p, vw.astype(F32))
    o = o.reshape(B, nb * C, H, dh)[:, :L]
    lse = lse.transpose(0, 1, 4, 2, 3).reshape(B, nb * C, H)[:, :L]
    return o, lse


def dilated_attention(q, k, v, window, dilation):
    B, S, H, dh = q.shape
    L = S // dilation

    def fold(a):
        return a.reshape(B, L, dilation, a.shape[2], dh).transpose(0, 2, 1, 3, 4).reshape(B * dilation, L, a.shape[2], dh)

    o, lse = banded_attention(fold(q), fold(k), fold(v), window // dilation)
    o = o.reshape(B, dilation, L, H, dh).transpose(0, 2, 1, 3, 4).reshape(B, S, H, dh)
    lse = lse.reshape(B, dilation, L, H).transpose(0, 2, 1, 3).reshape(B, S, H)
    return o, lse


def dilated_mixer(qkv_b, cos, sin):
    B, S, _ = qkv_b.shape
    qkv = qkv_b.reshape(B, S, 3, N_DIL_GROUPS, DIL_HEADS, HEAD_DIM)
    outs, lses = [], []
    for g, (w, d) in enumerate(DIL_PATTERNS):
        q = apply_rope(qkv[:, :, 0, g], cos, sin)
        k = apply_rope(qkv[:, :, 1, g], cos, sin)
        o, l = dilated_attention(q, k, qkv[:, :, 2, g], w, d)
        outs.append(o)
        lses.append(l)
    alpha = jax.nn.softmax(jnp.stack(lses, axis=0), axis=0)
    o = jnp.sum(alpha[..., None] * jnp.stack(outs, axis=0), axis=0)
    return o.reshape(B, S, OUT_B).astype(qkv_b.dtype)


def nsa_mixer(q, kv, gate_logits, cos, sin, pos_k, pos_v, wk1, wk2, wv1, wv2):
    B, S, _ = q.shape
    H, G, dh = NSA_HEADS, NSA_KV_HEADS, HEAD_DIM
    hpg = H // G
    scale = dh ** -0.5
    q = apply_rope(q.reshape(B, S, H, dh), cos, sin)
    kv = kv.reshape(B, S, 3, 2, G, dh)
    k_cmp, v_cmp = apply_rope(kv[:, :, 0, 0], cos, sin), kv[:, :, 0, 1]
    k_sel, v_sel = apply_rope(kv[:, :, 1, 0], cos, sin), kv[:, :, 1, 1]
    k_win, v_win = apply_rope(kv[:, :, 2, 0], cos, sin), kv[:, :, 2, 1]
    qg = q.reshape(B, S, G, hpg, dh).astype(F32)
    t = jnp.arange(S)

    n_c = (S - CMP_LEN) // CMP_STRIDE + 1
    starts = jnp.arange(n_c) * CMP_STRIDE
    idx = starts[:, None] + jnp.arange(CMP_LEN)[None, :]

    def compress(a, pos, w1, w2):
        blk = a[:, idx] + pos[None, None, :, None, :]
        blk = blk.transpose(0, 1, 3, 2, 4).reshape(B, n_c, G, CMP_LEN * dh)
        return jax.nn.silu(blk @ w1) @ w2

    kc = compress(k_cmp, pos_k, wk1, wk2)
    vc = compress(v_cmp, pos_v, wv1, wv2)
    s = jnp.einsum('bsghd,bcgd->bghsc', qg, kc.astype(F32)) * scale
    cmask = (starts + CMP_LEN - 1)[None, :] <= t[:, None]
    p_cmp = masked_softmax(s, cmask)
    o_cmp = jnp.einsum('bghsc,bcgd->bsghd', p_cmp, vc.astype(F32))

    nbs = S // SEL_LEN
    bstart = jnp.arange(nbs) * SEL_LEN
    overlap = jnp.clip(jnp.minimum(starts[:, None] + CMP_LEN, bstart[None, :] + SEL_LEN)
                       - jnp.maximum(starts[:, None], bstart[None, :]), 0)
    M = overlap.astype(F32) / CMP_LEN
    imp = jnp.einsum('bghsc,cj->bgsj', p_cmp, M)
    cur = (t // SEL_LEN)[:, None]
    j = jnp.arange(nbs)[None, :]
    forced = (j == 0) | (j == cur) | (j == cur - 1)
    imp = jnp.where(forced, jnp.inf, imp)
    imp = jnp.where(j > cur, -jnp.inf, imp)
    n_pick = min(N_SEL, nbs)
    _, sel_idx = lax.top_k(imp, n_pick)

    Cq = SEL_Q_BLOCK
    nq = S // Cq
    ksb = k_sel.reshape(B, nbs, SEL_LEN, G, dh).transpose(0, 3, 1, 2, 4)
    vsb = v_sel.reshape(B, nbs, SEL_LEN, G, dh).transpose(0, 3, 1, 2, 4)
    q_ch = qg.reshape(B, nq, Cq, G, hpg, dh).transpose(1, 0, 2, 3, 4, 5)
    idx_ch = sel_idx.reshape(B, G, nq, Cq, n_pick).transpose(2, 0, 1, 3, 4)
    t_ch = t.reshape(nq, Cq)
    b_ix = jnp.arange(B)[:, None, None, None]
    g_ix = jnp.arange(G)[None, :, None, None]
    Ksel = n_pick * SEL_LEN

    def sel_block(args):
        qc, ic, tc = args
        kg = ksb[b_ix, g_ix, ic].reshape(B, G, Cq, Ksel, dh)
        vg = vsb[b_ix, g_ix, ic].reshape(B, G, Cq, Ksel, dh)
        kpos = (ic[..., None] * SEL_LEN + jnp.arange(SEL_LEN)).reshape(B, G, Cq, Ksel)
        sc = jnp.einsum('bqghd,bgqkd->bghqk', qc, kg.astype(F32)) * scale
        m = (kpos <= tc[None, None, :, None])[:, :, None]
        p = jax.nn.softmax(jnp.where(m, sc, -jnp.inf), axis=-1)
        return jnp.einsum('bghqk,bgqkd->bqghd', p, vg.astype(F32))

    o_sel = lax.map(sel_block, (q_ch, idx_ch, t_ch))
    o_sel = o_sel.transpose(1, 0, 2, 3, 4, 5).reshape(B, S, H, dh)

    o_win, _ = banded_attention(q, k_win, v_win, WIN - 1)

    gates = jax.nn.sigmoid(gate_logits.astype(F32)).reshape(B, S, H, 3)
    o = (gates[..., 0:1] * o_cmp.reshape(B, S, H, dh)
         + gates[..., 1:2] * o_sel
         + gates[..., 2:3] * o_win)
    return o.reshape(B, S, OUT_A).astype(q.dtype)


def hybrid_mixer(h, cos, sin, w_in, pos_k, pos_v, wk1, wk2, wv1, wv2, p_a, p_b, w_o):
    B, S, D = h.shape
    proj = h @ w_in
    q_a, kv_a, g_a, qkv_b, merge = jnp.split(proj, SPLITS, axis=-1)
    y_a = nsa_mixer(q_a, kv_a, g_a, cos, sin, pos_k, pos_v, wk1, wk2, wv1, wv2) @ p_a
    y_b = dilated_mixer(qkv_b, cos, sin) @ p_b
    g = jax.nn.sigmoid(merge.astype(F32)).reshape(B, S, N_BRANCHES, D)
    y = (g[:, :, 0] * y_a.astype(F32) + g[:, :, 1] * y_b.astype(F32)).astype(h.dtype)
    return y @ w_o


def swiglu(h, w1, w3, w2):
    return (jax.nn.silu(h @ w1) * (h @ w3)) @ w2


def moe_swiglu(h, router, w1, w3, w2):
    B, S, D = h.shape
    x = h.reshape(B * S, D)
    logits = (x @ router).astype(F32)
    top_v, top_i = lax.top_k(logits, TOP_K)
    wts = jax.nn.softmax(top_v, axis=-1)
    combine = jnp.sum(jax.nn.one_hot(top_i, N_EXPERTS, dtype=F32) * wts[..., None], axis=1)
    out = jnp.zeros((B * S, D), F32)
    for e in range(N_EXPERTS):
        out = out + combine[:, e:e + 1] * swiglu(x, w1[e], w3[e], w2[e]).astype(F32)
    return out.reshape(B, S, D).astype(h.dtype)


def setup_inputs(seed: int = 0) -> dict:
    key = jax.random.key(seed)
    ks = iter(jax.random.split(key, 32))

    def nrm(shape, scale):
        return jax.random.normal(next(ks), shape, F32) * scale

    x = nrm((BATCH, SEQ, D_MODEL), 1.0)
    offset = jax.random.randint(next(ks), (BATCH, 1), 0, 4096, dtype=jnp.int32)
    positions = offset + jnp.arange(SEQ, dtype=jnp.int32)[None, :]
    return {
        "x": x,
        "positions": positions,
        "norm_mix": 1.0 + nrm((DEPTH, D_MODEL), 0.02),
        "w_in": nrm((DEPTH, D_MODEL, IN_COLS), D_MODEL ** -0.5),
        "cmp_pos_k": nrm((DEPTH, CMP_LEN, HEAD_DIM), 0.02),
        "cmp_pos_v": nrm((DEPTH, CMP_LEN, HEAD_DIM), 0.02),
        "cmp_k_w1": nrm((DEPTH, CMP_LEN * HEAD_DIM, CMP_HIDDEN), (CMP_LEN * HEAD_DIM) ** -0.5),
        "cmp_k_w2": nrm((DEPTH, CMP_HIDDEN, HEAD_DIM), CMP_HIDDEN ** -0.5),
        "cmp_v_w1": nrm((DEPTH, CMP_LEN * HEAD_DIM, CMP_HIDDEN), (CMP_LEN * HEAD_DIM) ** -0.5),
        "cmp_v_w2": nrm((DEPTH, CMP_HIDDEN, HEAD_DIM), CMP_HIDDEN ** -0.5),
        "w_branch_a": nrm((DEPTH, OUT_A, D_MODEL), OUT_A ** -0.5),
        "w_branch_b": nrm((DEPTH, OUT_B, D_MODEL), OUT_B ** -0.5),
        "w_out": nrm((DEPTH, D_MODEL, D_MODEL), D_MODEL ** -0.5),
        "norm_ffn": 1.0 + nrm((DEPTH, D_MODEL), 0.02),
        "ffn_w1": nrm((N_DENSE, D_MODEL, D_FF), D_MODEL ** -0.5),
        "ffn_w3": nrm((N_DENSE, D_MODEL, D_FF), D_MODEL ** -0.5),
        "ffn_w2": nrm((N_DENSE, D_FF, D_MODEL), D_FF ** -0.5),
        "router": nrm((N_MOE, D_MODEL, N_EXPERTS), D_MODEL ** -0.5),
        "moe_w1": nrm((N_MOE, N_EXPERTS, D_MODEL, D_FF), D_MODEL ** -0.5),
        "moe_w3": nrm((N_MOE, N_EXPERTS, D_MODEL, D_FF), D_MODEL ** -0.5),
        "moe_w2": nrm((N_MOE, N_EXPERTS, D_FF, D_MODEL), D_FF ** -0.5),
        "final_norm": 1.0 + nrm((D_MODEL,), 0.02),
    }


def reference(x, positions, norm_mix, w_in, cmp_pos_k, cmp_pos_v, cmp_k_w1, cmp_k_w2,
              cmp_v_w1, cmp_v_w2, w_branch_a, w_branch_b, w_out, norm_ffn, ffn_w1, ffn_w3,
              ffn_w2, router, moe_w1, moe_w3, moe_w2, final_norm):
    cos, sin = rope_tables(positions)
    for layer in range(DEPTH):
        h = rms_norm(x, norm_mix[layer])
        x = x + hybrid_mixer(h, cos, sin, w_in[layer], cmp_pos_k[layer], cmp_pos_v[layer],
                             cmp_k_w1[layer], cmp_k_w2[layer], cmp_v_w1[layer], cmp_v_w2[layer],
                             w_branch_a[layer], w_branch_b[layer], w_out[layer])
        h = rms_norm(x, norm_ffn[layer])
        i = layer // 2
        if layer % 2 == 0:
            x = x + swiglu(h, ffn_w1[i], ffn_w3[i], ffn_w2[i])
        else:
            x = x + moe_swiglu(h, router[i], moe_w1[i], moe_w3[i], moe_w2[i])
    return rms_norm(x, final_norm)
```

```python
import numpy as np
from contextlib import ExitStack
import concourse.bass as bass
import concourse.mybir as mybir
from concourse.bass_utils import run_bass_kernel_spmd

F32 = mybir.dt.float32
BF16 = mybir.dt.bfloat16
I32 = mybir.dt.int32
AF = mybir.ActivationFunctionType
ALU = mybir.AluOpType

GRAN = 256
N_DMA_SEM = 12
NEG = -30000.0
S = 2048
D = 1024
DFF = 2816
NJ = DFF // 128
IN_COLS = 5656
EPS = 1e-6


class Buf:
    def __init__(self, ap, lo, hi, space, shape=None, esz=2, exact=True):
        self.ap = ap
        self.lo = lo
        self.hi = hi
        self.space = space
        self.shape = shape
        self.esz = esz
        self.exact = exact and shape is not None

    def grans(self):
        if self.space == 'ps':
            return range((1 << 20) + self.lo // 2048, (1 << 20) + (self.hi - 1) // 2048 + 1)
        return range(self.lo // GRAN, (self.hi - 1) // GRAN + 1)

    def __getitem__(self, key):
        if not isinstance(key, tuple):
            key = (key,)
        ap = self.ap[key]
        if not self.exact:
            return Buf(ap, self.lo, self.hi, self.space, None, self.esz, False)
        key = key + (slice(None),) * (len(self.shape) - len(key))
        fs = self.shape[1:]
        strides = [1] * len(fs)
        for i in range(len(fs) - 2, -1, -1):
            strides[i] = strides[i + 1] * fs[i + 1]
        mn = mx = 0
        for i, k in enumerate(key[1:]):
            n = fs[i]
            if isinstance(k, int):
                a = b = k
            else:
                a, b, st = k.indices(n)
                b = a + ((b - a - 1) // st) * st
            mn += a * strides[i]
            mx += b * strides[i]
        return Buf(ap, self.lo + mn * self.esz, self.lo + (mx + 1) * self.esz, self.space, None, self.esz, False)


class Op:
    __slots__ = ("id", "eng", "emit", "deps", "is_dma", "val", "needed", "qeng")


class Prog:
    ENGS = ("pe", "act", "dve", "pool", "sp")

    def __init__(self):
        self.ops = []
        self.lastw = {}
        self.lastr = {}
        self.dma_rr = {"sp": 0, "pool": 0}
        self.dma_last = {"sp": [None] * N_DMA_SEM, "pool": [None] * N_DMA_SEM}

    def op(self, eng, emit, reads=(), writes=(), dma=False):
        o = Op()
        o.id = len(self.ops)
        o.emit = emit
        o.is_dma = dma
        o.qeng = eng
        o.needed = False
        o.val = None
        deps = set()
        if dma:
            k = self.dma_rr[eng]
            self.dma_rr[eng] = (k + 1) % N_DMA_SEM
            o.eng = ("d%d" if eng == "sp" else "g%d") % k
            if self.dma_last[eng][k] is not None:
                deps.add(self.dma_last[eng][k])
            self.dma_last[eng][k] = o.id
        else:
            o.eng = eng
        lw, lr = self.lastw, self.lastr
        for b in reads:
            for g in b.grans():
                w = lw.get(g)
                if w is not None:
                    deps.add(w)
        for b in writes:
            for g in b.grans():
                w = lw.get(g)
                if w is not None:
                    deps.add(w)
                rs = lr.get(g)
                if rs:
                    deps.update(rs.values())
        for b in reads:
            for g in b.grans():
                d = lr.get(g)
                if d is None:
                    lr[g] = {o.eng: o.id}
                else:
                    d[o.eng] = o.id
        for b in writes:
            for g in b.grans():
                lw[g] = o.id
                lr[g] = None
        deps.discard(o.id)
        o.deps = deps
        self.ops.append(o)
        return o

    def finalize(self, sems, dsems, block):
        ops = self.ops
        for o in ops:
            nd = set()
            for d in o.deps:
                do = ops[d]
                if do.eng == o.eng and o.eng == "pe":
                    continue
                nd.add(d)
                do.needed = True
            o.deps = nd
        cnt = {}
        for o in ops:
            if o.is_dma:
                o.needed = True
            if o.needed:
                step = 16 if o.is_dma else 1
                cnt[o.eng] = cnt.get(o.eng, 0) + step
                o.val = cnt[o.eng]
        self.final_counts = cnt
        queues = {e: [] for e in self.ENGS}
        for o in ops:
            queues[o.qeng].append(o)

        def semof(k):
            if k[0] == "d" and k[1:].isdigit():
                return dsems[int(k[1:])]
            if k[0] == "g" and k[1:].isdigit():
                return dsems[N_DMA_SEM + int(k[1:])]
            return sems[k]

        def run_queue(qname, engobj):
            waited = {}
            for o in queues[qname]:
                need = {}
                for d in o.deps:
                    do = ops[d]
                    if do.val > need.get(do.eng, 0):
                        need[do.eng] = do.val
                for ek, v in need.items():
                    if waited.get(ek, 0) >= v:
                        continue
                    engobj.wait_ge(semof(ek), v)
                    waited[ek] = v
                ins = o.emit(engobj)
                if o.needed:
                    ins.then_inc(semof(o.eng), 16 if o.is_dma else 1)

        @block.tensor
        def _(e):
            run_queue("pe", e)

        @block.scalar
        def _(e):
            run_queue("act", e)

        @block.vector
        def _(e):
            run_queue("dve", e)

        @block.gpsimd
        def _(e):
            run_queue("pool", e)

        @block.sync
        def _(e):
            run_queue("sp", e)


def host_consts():
    import ml_dtypes
    bf = ml_dtypes.bfloat16
    c = {}
    c["c_ident_bf"] = np.eye(128, dtype=np.float32).astype(bf)
    c["c_ident_f"] = np.eye(128, dtype=np.float32)
    c["c_ones_f"] = np.full((128, 128), 1.0 / D, dtype=np.float32).astype(bf)
    c["c_ones1_f"] = np.ones((128, 128), dtype=np.float32).astype(bf)
    rm = np.zeros((128, 128), np.float32)
    for m in range(128):
        r = m % 64
        if r < 8:
            rm[m + 8, m] = -1.0
        elif r < 16:
            rm[m - 8, m] = 1.0
    c["c_rmat"] = rm.astype(bf)
    inv = (500000.0 ** (-np.arange(0, 16, 2, dtype=np.float32) / 16.0)).astype(np.float32)
    invrow = np.zeros((128, 1), np.float32)
    for p in range(128):
        if p % 64 < 16:
            invrow[p, 0] = inv[p % 8]
    c["c_invrow"] = invrow
    k = np.arange(128)[:, None]

    def wtab(width, f):
        r = np.arange(width)[None, :]
        dl = r - 384 - k
        return np.where(f(dl), 0.0, NEG).astype(np.float32).astype(bf)

    c["c_wwin"] = wtab(1408, lambda d: (d >= 0) & (d <= 511))
    c["c_wcaus"] = wtab(896, lambda d: d >= 0)
    c["c_wg1"] = wtab(1024, lambda d: (d >= 0) & (d <= 128))
    c["c_wg2"] = wtab(1408, lambda d: (d >= 0) & (d <= 512) & (d % 4 == 0))
    c["c_wg3"] = wtab(1024, lambda d: (d >= 0) & (d % 16 == 0))
    t = np.arange(S)[None, :]
    cm = np.where((16 * k + 31 <= t) & (k < 127), 0.0, NEG).astype(np.float32)
    c["c_cmpmask"] = cm.astype(bf)
    E = np.zeros((32, S), np.float32)
    for j in range(32):
        E[j, 64 * j:64 * j + 64] = 1.0
    c["c_E"] = E.astype(bf)
    starts = np.arange(127) * 16
    bstart = np.arange(32) * 64
    ov = np.clip(np.minimum(starts[:, None] + 32, bstart[None, :] + 64) - np.maximum(starts[:, None], bstart[None, :]), 0, None)
    M = np.zeros((128, 33), np.float32)
    M[:127, :32] = ov / 32.0
    M[:, 32] = 1.0
    c["c_maug"] = M.astype(bf)
    fb = np.zeros((128, 16, 32), np.float32)
    for tile in range(16):
        for p in range(128):
            tt = tile * 128 + p
            cur = tt // 64
            for j in range(32):
                if j > cur:
                    v = -1e9
                elif j == 0:
                    v = 3e9
                elif j == cur:
                    v = 2e9
                elif j == cur - 1:
                    v = 1e9
                else:
                    v = 0.0
                fb[p, tile, j] = v
    c["c_forceb"] = fb
    sg = np.zeros((24, 12, 128), np.float32)
    for ch in range(4):
        for br in range(3):
            for m in range(128):
                sg[3 * (2 * ch + m // 64) + br, ch * 3 + br, m] = 1.0
    c["c_selgate"] = sg.astype(bf)
    return c


CONST_SHAPES = None


def build_program(stop_after="full"):
    nc = bass.Bass("TRN2", target_bir_lowering=False)
    P = Prog()
    hc = host_consts()

    def din(name, shape, dt=F32):
        return nc.dram_tensor(name, list(shape), dt, kind="ExternalInput").ap()

    dr = {}
    dr["xT"] = din("xT", [D, S])
    dr["pos"] = din("pos", [1, S], I32)
    for k_, v_ in hc.items():
        dr[k_] = din(k_, v_.shape, F32 if v_.dtype == np.float32 else BF16)
    for l in range(2):
        dr["w_in%d" % l] = din("w_in%d" % l, [D, IN_COLS])
        dr["g_mix%d" % l] = din("g_mix%d" % l, [D])
        dr["g_ffn%d" % l] = din("g_ffn%d" % l, [D])
        dr["posk%d" % l] = din("posk%d" % l, [32, 64])
        dr["posv%d" % l] = din("posv%d" % l, [32, 64])
        dr["kw1_%d" % l] = din("kw1_%d" % l, [2048, 128])
        dr["kw2_%d" % l] = din("kw2_%d" % l, [128, 64])
        dr["vw1_%d" % l] = din("vw1_%d" % l, [2048, 128])
        dr["vw2_%d" % l] = din("vw2_%d" % l, [128, 64])
        dr["pa%d" % l] = din("pa%d" % l, [512, D])
        dr["pb%d" % l] = din("pb%d" % l, [256, D])
        dr["wo%d" % l] = din("wo%d" % l, [D, D])
    _st = ["mixer0", "ffn0", "mixer1", "ffn1", "full"]
    _si = _st.index(stop_after) if stop_after in _st else 0
    if _si >= 1:
        dr["f_w1"] = din("f_w1", [1, D, DFF])
        dr["f_w3"] = din("f_w3", [1, D, DFF])
        dr["f_w2"] = din("f_w2", [1, DFF, D])
    if _si >= 3:
        dr["router"] = din("router", [D, 8])
        dr["m_w1"] = din("m_w1", [8, D, DFF])
        dr["m_w3"] = din("m_w3", [8, D, DFF])
        dr["m_w2"] = din("m_w2", [8, DFF, D])
    dr["g_fin"] = din("g_fin", [D])
    outT = nc.dram_tensor("outT", [D, S], F32, kind="ExternalOutput").ap()

    ARENA_BYTES = 212480
    with ExitStack() as st:
        arena = st.enter_context(nc.sbuf_tensor("arena", [128, ARENA_BYTES // 2], BF16))
        pst = [st.enter_context(nc.psum_tensor("ps%d" % i, [128, 512], F32)) for i in range(8)]
        sems = {e: st.enter_context(nc.semaphore("s_" + e)) for e in Prog.ENGS}
        dsems = [st.enter_context(nc.semaphore("dq%d" % i)) for i in range(2 * N_DMA_SEM)]
        block = st.enter_context(nc.Block())

        top = [0]

        def alloc(shape, dt):
            n = int(np.prod(shape[1:]))
            esz = 2 if dt == BF16 else 4
            nb = n * esz
            lo = (top[0] + 255) // 256 * 256
            assert lo + nb <= ARENA_BYTES, ("SBUF overflow", lo + nb)
            top[0] = lo + nb
            v = arena[:, lo // 2:(lo + nb) // 2]
            if dt != BF16:
                v = v.bitcast(dt)
            if len(shape) == 3:
                v = v.rearrange("p (a b) -> p a b", b=shape[2])
            elif len(shape) == 4:
                v = v.rearrange("p (a b c) -> p a b c", b=shape[2], c=shape[3])
            v = v[0:shape[0]]
            return Buf(v, lo, lo + nb, 'sb', tuple(shape), esz)

        PS = [Buf(pst[i][:], i * 2048, (i + 1) * 2048, 'ps', (128, 512), 4) for i in range(8)]
        rr = {"S": 0, "O": 0, "M": 0, "R": 0}

        def ps_next(kind):
            banks = {"S": (0, 1, 2), "O": (3, 4, 5), "M": (6, 7), "R": (0, 1, 2, 3, 4, 5)}[kind]
            i = rr[kind]
            rr[kind] = (i + 1) % len(banks)
            return PS[banks[i]]

        def bufs_of(*xs):
            return [x for x in xs if isinstance(x, Buf)]

        def sc(x):
            return x.ap if isinstance(x, Buf) else x

        def mm(out, lhsT, rhs, start, stop):
            P.op("pe", lambda e: e.matmul(out.ap, lhsT=lhsT.ap, rhs=rhs.ap, start=start, stop=stop,
                                          skip_group_check=True), reads=[lhsT, rhs], writes=[out])

        def act(out, in_, func, scale=1.0, bias=None):
            if bias is None:
                P.op("act", lambda e: e.activation(out=out.ap, in_=in_.ap, func=func, scale=scale),
                     reads=[in_], writes=[out])
            else:
                P.op("act", lambda e: e.activation(out=out.ap, in_=in_.ap, func=func, scale=scale, bias=bias.ap),
                     reads=[in_, bias], writes=[out])

        def tcopy(eng, out, in_):
            P.op(eng, lambda e: e.tensor_copy(out=out.ap, in_=in_.ap), reads=[in_], writes=[out])

        def tt(eng, out, a, b, op):
            P.op(eng, lambda e: e.tensor_tensor(out=out.ap, in0=a.ap, in1=b.ap, op=op), reads=[a, b], writes=[out])

        def ts(eng, out, a, s1, s2, op0, op1=None):
            if op1 is None:
                P.op(eng, lambda e: e.tensor_scalar(out=out.ap, in0=a.ap, scalar1=sc(s1), scalar2=None, op0=op0),
                     reads=[a] + bufs_of(s1), writes=[out])
            else:
                P.op(eng, lambda e: e.tensor_scalar(out=out.ap, in0=a.ap, scalar1=sc(s1), scalar2=sc(s2), op0=op0, op1=op1),
                     reads=[a] + bufs_of(s1, s2), writes=[out])

        def stt(eng, out, a, s, b, op0, op1):
            P.op(eng, lambda e: e.scalar_tensor_tensor(out=out.ap, in0=a.ap, scalar=sc(s), in1=b.ap, op0=op0, op1=op1),
                 reads=[a, b] + bufs_of(s), writes=[out])

        def recip(out, in_):
            P.op("dve", lambda e: e.reciprocal(out=out.ap, in_=in_.ap), reads=[in_], writes=[out])

        def memset(eng, b, val):
            P.op(eng, lambda e: e.memset(b.ap, val), writes=[b])

        def dma(q, out_buf, in_ap, **kw):
            return P.op(q, lambda e: e.dma_start(out=out_buf.ap, in_=in_ap, **kw), writes=[out_buf], dma=True)

        def wload(out_buf, in_ap, **kw):
            return dma("pool", out_buf, in_ap, **kw)

        xT = alloc([128, 8, S], F32)
        hT = alloc([128, 8, S], BF16)
        ident_bf = alloc([128, 128], BF16)
        ident_f = alloc([128, 128], F32)
        ones_f = alloc([128, 128], BF16)
        rmat = alloc([128, 128], BF16)
        invrow = alloc([128, 1], F32)
        gains = alloc([128, 5, 8], F32)
        posi = None
        dma("sp", ident_bf, dr["c_ident_bf"])
        dma("sp", ident_f, dr["c_ident_f"])
        dma("sp", ones_f, dr["c_ones_f"])
        dma("sp", rmat, dr["c_rmat"])
        dma("sp", invrow, dr["c_invrow"])
        for i, nm in enumerate(["g_mix0", "g_ffn0", "g_mix1", "g_ffn1", "g_fin"]):
            dma("sp", gains[:, i, :], dr[nm].rearrange("(c p) -> p c", p=128), allow_slow_non_contiguous=True)
        for f in range(8):
            dma("sp", xT[:, f, :], dr["xT"][f * 128:(f + 1) * 128, :])
        PH0 = top[0]

        def cs(tq):
            return slice(tq * 512, (tq + 1) * 512)

        def rmsnorm(gain_idx, rstd_keep=None):
            m0 = top[0]
            sq = [alloc([128, 512], F32) for _ in range(2)]
            sqh = [alloc([128, 512], BF16) for _ in range(2)]
            sql = [alloc([128, 512], BF16) for _ in range(2)]
            rs_tmp = [alloc([128, 512], F32) for _ in range(2)]
            for tq in range(4):
                pm = ps_next("M")
                for f in range(8):
                    s_ = sq[f % 2]
                    act(s_, xT[:, f, cs(tq)], AF.Square)
                    tcopy("dve", sqh[f % 2], s_)
                    tt("dve", sql[f % 2], s_, sqh[f % 2], ALU.subtract)
                    mm(pm, ones_f, sqh[f % 2], f == 0, False)
                    mm(pm, ones_f, sql[f % 2], False, f == 7)
                rstd = rstd_keep[tq] if rstd_keep is not None else rs_tmp[tq % 2]
                ts("dve", rstd, pm, EPS, None, ALU.add)
                act(rstd, rstd, AF.Sqrt)
                recip(rstd, rstd)
                for f in range(8):
                    stt("dve", hT[:, f, cs(tq)], xT[:, f, cs(tq)], gains[:, gain_idx, f:f + 1], rstd, ALU.mult, ALU.mult)
            top[0] = m0

        def proj_fm(wt, wsl, tq, ps):
            for d in range(8):
                mm(ps, wt[:, d, wsl], hT[:, d, cs(tq)], d == 0, d == 7)

        def rope_evac(ps, dst, cosT, sinT, tq, tmps):
            import os as _os
            lvl = int(_os.environ.get("DBG_ROPE", "5"))
            raw, t1, t2 = tmps
            if lvl >= 1:
                tcopy("dve", raw, ps)
            if lvl >= 2:
                pp = ps_next("M")
                mm(pp, rmat, raw, True, True)
            if lvl >= 3:
                tt("dve", t1, ps, cosT[:, cs(tq)], ALU.mult)
            if lvl >= 4:
                tt("dve", t2, pp, sinT[:, cs(tq)], ALU.mult)
            if lvl >= 5:
                tt("dve", dst, t1, t2, ALU.add)
            else:
                tcopy("dve", dst, ps)

        def proj_rope(w, dst, cosT, sinT, rtmps):
            pks = []
            for tq_ in range(4):
                pk_ = ps_next("R")
                proj_fm(w, slice(0, 128), tq_, pk_)
                pks.append(pk_)
            for tq_ in range(4):
                rope_evac(pks[tq_], dst[:, cs(tq_)], cosT, sinT, tq_, rtmps)

        def attn_run(calls, ptiles, pidx):
            flat = []
            for ci, (steps, fin) in enumerate(calls):
                fi = [i for i, s_ in enumerate(steps) if s_["j0"] == 0 and s_["j1"] == 512][0]
                steps = [steps[fi]] + steps[:fi] + steps[fi + 1:]
                for si, stp in enumerate(steps):
                    flat.append((ci, si, len(steps), stp, fin))

            def emit_qk(stp):
                j0, j1 = stp["j0"], stp["j1"]
                pS = ps_next("S")
                ml = stp["masks"]
                mm(pS[:, j0:j1], stp["k"], stp["q"], True, len(ml) == 0)
                for mi, (a_, b_) in enumerate(ml):
                    mm(pS[:, j0:j1], a_, b_, False, mi == len(ml) - 1)
                return pS

            LOOK = 2
            pos = {}
            pend = []
            nq = 0
            while nq < min(LOOK, len(flat)):
                pend.append(emit_qk(flat[nq][3]))
                nq += 1
            for gi, (ci, si, n, stp, fin) in enumerate(flat):
                pS = pend.pop(0)
                if nq < len(flat):
                    pend.append(emit_qk(flat[nq][3]))
                    nq += 1
                if si == 0:
                    pos[ci] = ps_next("O")
                po = pos[ci]
                j0, j1 = stp["j0"], stp["j1"]
                pt = ptiles[pidx[0] % len(ptiles)]
                pidx[0] += 1
                act(pt[:, j0:j1], pS[:, j0:j1], AF.Exp, scale=0.125)
                mm(po[:, j0:j1], stp["v"], pt[:, j0:j1], si == 0, si == n - 1)
                if si == n - 1:
                    fin(po)

        class StopBuild(Exception):
            pass

        def chk(name):
            if stop_after == name:
                raise StopBuild()

        def mixer(l):
            m_layer = top[0]
            w_in = dr["w_in%d" % l].rearrange("(c p) n -> p c n", p=128)
            cosT = alloc([128, S], F32)
            sinT = alloc([128, S], F32)
            m1 = top[0]
            posi_ = alloc([128, S], I32)
            ang = alloc([128, S], F32)
            wv_ = alloc([128, S], F32)
            mk_ = alloc([128, S], F32)
            dma("sp", posi_, dr["pos"].partition_broadcast(128))
            tcopy("dve", ang, posi_)
            ts("dve", ang, ang, invrow, float(1.0 / (2 * np.pi)), ALU.mult, ALU.mult)
            for (dst, shift) in ((sinT, 0.0), (cosT, 0.25)):
                if shift != 0.0:
                    ts("dve", ang, ang, shift, None, ALU.add)
                tcopy("dve", posi_, ang)
                tcopy("dve", wv_, posi_)
                tt("dve", wv_, ang, wv_, ALU.subtract)
                ts("dve", mk_, wv_, 0.5, None, ALU.is_gt)
                tt("dve", wv_, wv_, mk_, ALU.subtract)
                ts("dve", mk_, wv_, -0.5, None, ALU.is_lt)
                tt("dve", wv_, wv_, mk_, ALU.add)
                act(dst, wv_, AF.Sin, scale=6.28318)
            top[0] = m1

            rmsnorm(2 * l)
            if stop_after == "m_pre":
                top[0] = m_layer
                return

            oa = [alloc([128, S], BF16) for _ in range(4)]
            ob = [alloc([128, S], BF16) for _ in range(2)]
            m_attn = top[0]

            wwin = alloc([128, 1408], BF16)
            wcaus = alloc([128, 896], BF16)
            cmpmask = alloc([128, S], BF16)
            Emat = alloc([32, S], BF16)
            maug = alloc([128, 33], BF16)
            forceb = alloc([128, 16, 32], F32)
            selgate = alloc([24, 12, 128], BF16)
            for b_, nm in ((wwin, "c_wwin"), (wcaus, "c_wcaus"), (cmpmask, "c_cmpmask"), (Emat, "c_E"),
                           (maug, "c_maug"), (forceb, "c_forceb"), (selgate, "c_selgate")):
                dma("sp", b_, dr[nm])
            GT = alloc([24, S], BF16)
            ptiles = [alloc([128, 512], BF16) for _ in range(3)]
            raw = alloc([128, 512], BF16)
            t1 = alloc([128, 512], F32)
            t2 = alloc([128, 512], F32)
            rtmps = (raw, t1, t2)
            rd = t1
            on = t2
            wst = [alloc([128, 8, 128], BF16) for _ in range(2)]
            wsi = [0]

            def next_w():
                w = wst[wsi[0] % len(wst)]
                wsi[0] += 1
                return w

            wg = next_w()
            wload(wg, w_in[:, :, 1280:1408])
            for tq in range(4):
                pg = ps_next("M")
                for d in range(8):
                    mm(pg[0:24, :], wg[:, d, 0:24], hT[:, d, cs(tq)], d == 0, d == 7)
                act(GT[:, cs(tq)], pg[0:24, :], AF.Sigmoid)
            chk("n0")
            kcT2 = [alloc([128, 128], BF16) for _ in range(2)]
            vaug_cmp = [alloc([128, 128], BF16) for _ in range(2)]
            m_grp = top[0]
            kcmpT = alloc([128, S], BF16)
            vcmpT = alloc([128, S], BF16)
            w1k = alloc([128, 32, 128], BF16)
            w1v = alloc([128, 32, 128], BF16)
            w2k = alloc([128, 128], BF16)
            w2v = alloc([128, 64], BF16)
            pkT = alloc([128, 32], BF16)
            pvT = alloc([128, 32], BF16)
            biask = alloc([128, 1], F32)
            biasv = alloc([128, 1], F32)
            hsk = alloc([128, 128], BF16)
            hsv = alloc([128, 128], BF16)
            wk_ = next_w()
            wload(wk_, w_in[:, :, 512:640])
            proj_rope(wk_, kcmpT, cosT, sinT, rtmps)
            chk("p1")
            wv = next_w()
            wload(wv, w_in[:, :, 640:768])
            for tq in range(4):
                pv = ps_next("M")
                proj_fm(wv, slice(0, 128), tq, pv)
                tcopy("dve", vcmpT[:, cs(tq)], pv)
            chk("p2")
            for half in range(2):
                sl = slice(half * 64, half * 64 + 64)
                for l4 in range(4):
                    wload(w1k[sl, l4 * 8:(l4 + 1) * 8, :], dr["kw1_%d" % l].rearrange("(l d) h -> d l h", d=64)[:, l4 * 8:(l4 + 1) * 8, :])
                    wload(w1v[sl, l4 * 8:(l4 + 1) * 8, :], dr["vw1_%d" % l].rearrange("(l d) h -> d l h", d=64)[:, l4 * 8:(l4 + 1) * 8, :])
            chk("p3")
            w2st = alloc([128, 128], F32)
            dma("sp", w2st[:, 0:64], dr["kw2_%d" % l])
            dma("sp", w2st[:, 64:128], dr["vw2_%d" % l])
            tcopy("dve", w2k[:, 0:64], w2st[:, 0:64])
            tcopy("dve", w2k[:, 64:128], w2st[:, 0:64])
            tcopy("dve", w2v, w2st[:, 64:128])
            chk("p4")
            posn = alloc([32, 128], F32)
            dma("sp", posn[:, 0:64], dr["posk%d" % l])
            dma("sp", posn[:, 64:128], dr["posv%d" % l])
            posnb = alloc([32, 128], BF16)
            tcopy("dve", posnb, posn)
            for (pxT, c0_) in ((pkT, 0), (pvT, 64)):
                ptp = ps_next("M")
                mm(ptp[0:64, 0:32], posnb[:, c0_:c0_ + 64], ident_bf[0:32, 0:32], True, True)
                tcopy("dve", pxT[0:64, :], ptp[0:64, 0:32])
            chk("n1")
            for (w1x, pxT, bx) in ((w1k, pkT, biask), (w1v, pvT, biasv)):
                pb_ = ps_next("M")
                for li in range(32):
                    mm(pb_[:, 0:1], w1x[0:64, li, :], pxT[0:64, li:li + 1], li == 0, li == 31)
                tcopy("dve", bx, pb_[:, 0:1])
            for g in range(2):
                gs = slice(g * 64, g * 64 + 64)
                memset("pool", kcT2[g], 0.0)
                memset("pool", vaug_cmp[g], 0.0)
                memset("pool", vaug_cmp[g][:, 64:128], 1.0)
                ph = ps_next("M")
                for li in range(32):
                    mm(ph[:, 0:127], w1k[gs, li, :], kcmpT[gs, li:li + 16 * 126 + 1:16], li == 0, li == 31)
                act(hsk[:, 0:127], ph[:, 0:127], AF.Silu, bias=biask)
                pk2 = ps_next("M")
                mm(pk2[:, 0:127], w2k, hsk[:, 0:127], True, True)
                tcopy("dve", kcT2[g][:, 0:127], pk2[:, 0:127])
                ph = ps_next("M")
                for li in range(32):
                    mm(ph[:, 0:127], w1v[gs, li, :], vcmpT[gs, li:li + 16 * 126 + 1:16], li == 0, li == 31)
                act(hsv[:, 0:127], ph[:, 0:127], AF.Silu, bias=biasv)
                pv2 = ps_next("M")
                mm(pv2[0:127, 0:64], hsv[:, 0:127], w2v, True, True)
                tcopy("dve", vaug_cmp[g][0:127, 0:64], pv2[0:127, 0:64])

            chk("n2")
            for g in range(2):
                top[0] = m_grp
                kT = [None] + [alloc([128, S], BF16) for _ in range(2)]
                qT1 = alloc([128, S], BF16)
                vaug_sel = alloc([128, 16, 128], BF16)
                vaug_win = alloc([128, 16, 128], BF16)
                imp = alloc([128, 16, 32], F32)
                negsel = alloc([128, 32], BF16)
                negselT = alloc([32, S], BF16)
                m8 = alloc([128, 16], F32)
                mrep = alloc([128, 32], F32)
                rdn = alloc([128, 4], F32)
                m8q = [alloc([128, 16], F32) for _ in range(4)]
                mrepq = [alloc([128, 32], F32) for _ in range(4)]
                negselq = [alloc([128, 32], BF16) for _ in range(4)]
                rdnq = [alloc([128, 1], F32) for _ in range(4)]
                wdup = alloc([128, 8, 128], BF16)
                for bi in (1, 2):
                    col = 512 + bi * 256 + g * 64
                    wl_ = next_w()
                    col0 = 512 + bi * 256
                    wload(wl_, w_in[:, :, col0:col0 + 128])
                    w = wdup
                    tcopy("dve", w[:, :, 0:64], wl_[:, :, g * 64:g * 64 + 64])
                    tcopy("dve", w[:, :, 64:128], wl_[:, :, g * 64:g * 64 + 64])
                    proj_rope(w, kT[bi], cosT, sinT, rtmps)
                w = wdup
                wl_ = next_w()
                wload(wl_, w_in[:, :, 896:1024])
                tcopy("dve", w[:, :, 0:64], wl_[:, :, g * 64:g * 64 + 64])
                wl_ = next_w()
                wload(wl_, w_in[:, :, 1152:1280])
                tcopy("dve", w[:, :, 64:128], wl_[:, :, g * 64:g * 64 + 64])
                memset("pool", vaug_sel[:, :, 64:128], 1.0)
                memset("pool", vaug_win[:, :, 64:128], 1.0)
                for t4 in range(4):
                    pv = ps_next("M")
                    for tl in range(4):
                        ti = t4 * 4 + tl
                        for d in range(8):
                            mm(pv[:, tl * 128:(tl + 1) * 128], hT[:, d, ti * 128:(ti + 1) * 128], w[:, d, :], d == 0, d == 7)
                    pv3 = Buf(pv.ap.rearrange("p (a b) -> p a b", b=128), pv.lo, pv.hi, 'ps')
                    tcopy("dve", vaug_sel[:, t4 * 4:(t4 + 1) * 4, 0:64], pv3[:, :, 0:64])
                    tcopy("dve", vaug_win[:, t4 * 4:(t4 + 1) * 4, 0:64], pv3[:, :, 64:128])

                def proj_q(cc):
                    c_ = 2 * g + cc
                    w_ = next_w()
                    wload(w_, w_in[:, :, c_ * 128:(c_ + 1) * 128])
                    proj_rope(w_, qT1, cosT, sinT, rtmps)
                chk("n3")
                tcopy("pool", imp, forceb)

                def finish(po, c, hh, br, tq, first):
                    hs_ = slice(hh * 64, hh * 64 + 64)
                    ts("dve", rd[64:128, :], po[64:128, :], 1e-30, None, ALU.max)
                    recip(rd[64:128, :], rd[64:128, :])
                    tt("dve", on[hs_, :], po[0:64, :], rd[64:128, :], ALU.mult)
                    pg_ = ps_next("M")
                    mm(pg_, selgate[:, c * 3 + br, :], GT[:, cs(tq)], True, True)
                    if first:
                        tt("dve", oa[c][hs_, cs(tq)], on[hs_, :], pg_[hs_, :], ALU.mult)
                    else:
                        tt("dve", on[hs_, :], on[hs_, :], pg_[hs_, :], ALU.mult)
                        tt("dve", oa[c][hs_, cs(tq)], oa[c][hs_, cs(tq)], on[hs_, :], ALU.add)

                pidx = [0]
                for i in range(4):
                    cc, hh = i // 2, i % 2
                    c = 2 * g + cc
                    hs_ = slice(hh * 64, hh * 64 + 64)
                    if hh == 0:
                        proj_q(cc)
                    for tq in range(4):
                        pS = ps_next("S")
                        mm(pS, kcT2[g][hs_, :], qT1[hs_, cs(tq)], True, False)
                        mm(pS, ident_bf, cmpmask[:, cs(tq)], False, True)
                        pt = ptiles[pidx[0] % 3]
                        pidx[0] += 1
                        act(pt, pS, AF.Exp, scale=0.125)
                        po = ps_next("O")
                        mm(po, vaug_cmp[g], pt, True, True)
                        pI = ps_next("M")
                        for ql in range(4):
                            mm(pI[:, ql * 33:(ql + 1) * 33], pt[:, ql * 128:(ql + 1) * 128], maug, True, True)
                        for ql in range(4):
                            ts("dve", rdnq[ql], pI[:, ql * 33 + 32:ql * 33 + 33], 1e-30, None, ALU.max)
                        for ql in range(4):
                            recip(rdnq[ql], rdnq[ql])
                        for ql in range(4):
                            qt_ = tq * 4 + ql
                            stt("dve", imp[:, qt_, :], pI[:, ql * 33:ql * 33 + 32], rdnq[ql], imp[:, qt_, :], ALU.mult, ALU.add)
                        finish(po, c, hh, 0, tq, True)
                chk("n4")
                for tq in range(4):
                    pT = ps_next("M")
                    for ql in range(4):
                        qt_ = tq * 4 + ql
                        P.op("dve", lambda e, qt_=qt_, ql=ql: e.max(out=m8q[ql].ap[:, 0:8], in_=imp.ap[:, qt_, :]), reads=[imp[:, qt_, :]], writes=[m8q[ql]])
                    for ql in range(4):
                        qt_ = tq * 4 + ql
                        P.op("dve", lambda e, qt_=qt_, ql=ql: e.match_replace(out=mrepq[ql].ap, in_to_replace=m8q[ql].ap[:, 0:8], in_values=imp.ap[:, qt_, :], imm_value=-3e9),
                             reads=[imp[:, qt_, :], m8q[ql]], writes=[mrepq[ql]])
                    for ql in range(4):
                        P.op("dve", lambda e, ql=ql: e.max(out=m8q[ql].ap[:, 8:16], in_=mrepq[ql].ap), reads=[mrepq[ql]], writes=[m8q[ql]])
                    for ql in range(4):
                        qt_ = tq * 4 + ql
                        ts("dve", negselq[ql], imp[:, qt_, :], m8q[ql][:, 15:16], None, ALU.is_lt)
                    for ql in range(4):
                        ts("dve", negselq[ql], negselq[ql], NEG, None, ALU.mult)
                    for ql in range(4):
                        mm(pT[0:32, ql * 128:(ql + 1) * 128], negselq[ql], ident_bf, True, True)
                    tcopy("dve", negselT[:, cs(tq)], pT[0:32, :])
                chk("n5")
                for i in range(4):
                    cc, hh = i // 2, i % 2
                    c = 2 * g + cc
                    hs_ = slice(hh * 64, hh * 64 + 64)
                    if hh == 0:
                        proj_q(cc)
                        calls = []
                    for tq in range(4):
                        steps = []
                        kts = list(range(4 * tq + 4))
                        first = 4 * tq
                        kts = [first] + [k_ for k_ in kts if k_ != first]
                        for kt in kts:
                            dl = 512 * tq - 128 * kt
                            j0 = max(0, -dl)
                            j1 = 512
                            q0 = tq * 512
                            masks = [(Emat[:, kt * 128:(kt + 1) * 128], negselT[:, q0 + j0:q0 + j1])]
                            if kt >= 4 * tq:
                                masks.append((ident_bf, wcaus[:, dl + 384 + j0:dl + 384 + j1]))
                            steps.append(dict(j0=j0, j1=j1, k=kT[1][hs_, kt * 128:(kt + 1) * 128],
                                              q=qT1[hs_, q0 + j0:q0 + j1], v=vaug_sel[:, kt, :], masks=masks))
                        calls.append((steps, lambda po, c=c, hh=hh, tq=tq: finish(po, c, hh, 1, tq, False)))
                        steps = []
                        kts = list(range(max(0, 4 * tq - 4), 4 * tq + 4))
                        kts = [first] + [k_ for k_ in kts if k_ != first]
                        for kt in kts:
                            dl = 512 * tq - 128 * kt
                            j0 = max(0, -dl)
                            j1 = min(512, 639 - dl)
                            q0 = tq * 512
                            masks = [(ident_bf, wwin[:, dl + 384 + j0:dl + 384 + j1])]
                            steps.append(dict(j0=j0, j1=j1, k=kT[2][hs_, kt * 128:(kt + 1) * 128],
                                              q=qT1[hs_, q0 + j0:q0 + j1], v=vaug_win[:, kt, :], masks=masks))
                        calls.append((steps, lambda po, c=c, hh=hh, tq=tq: finish(po, c, hh, 2, tq, False)))
                    if hh == 1:
                        attn_run(calls, ptiles, pidx)

            if stop_after == "m_nsa":
                top[0] = m_layer
                return
            top[0] = m_attn
            wg1 = alloc([128, 1024], BF16)
            wg2 = alloc([128, 1408], BF16)
            wg3 = alloc([128, 1024], BF16)
            for b_, nm in ((wg1, "c_wg1"), (wg2, "c_wg2"), (wg3, "c_wg3")):
                dma("sp", b_, dr[nm])
            ptiles = [alloc([128, 512], BF16) for _ in range(3)]
            raw = alloc([128, 512], BF16)
            t1 = alloc([128, 512], F32)
            t2 = alloc([128, 512], F32)
            rtmps = (raw, t1, t2)
            rd = t1
            wst = [alloc([128, 8, 128], BF16) for _ in range(2)]
            wsi = [0]
            qb = [alloc([128, S], BF16) for _ in range(3)]
            kb = [alloc([128, S], BF16) for _ in range(3)]
            vb = [[alloc([128, 16, 128], BF16) for _ in range(2)] for _ in range(3)]
            pidx = [0]
            WTAB = (wg1, wg2, wg3)
            WIN = (128, 512, 2048)
            for hp in range(2):
                for g in range(3):
                    for kind, dstl in ((0, qb), (1, kb)):
                        col = 1304 + kind * 768 + g * 256 + hp * 128
                        w = next_w()
                        wload(w, w_in[:, :, col:col + 128])
                        proj_rope(w, dstl[g], cosT, sinT, rtmps)
                    col = 1304 + 2 * 768 + g * 256 + hp * 128
                    w = next_w()
                    wload(w, w_in[:, :, col:col + 128])
                    for hh in range(2):
                        memset("pool", vb[g][hh][:, :, 64:128], 1.0)
                    for t4 in range(4):
                        pv = ps_next("M")
                        for tl in range(4):
                            ti = t4 * 4 + tl
                            for d in range(8):
                                mm(pv[:, tl * 128:(tl + 1) * 128], hT[:, d, ti * 128:(ti + 1) * 128], w[:, d, :], d == 0, d == 7)
                        pv3 = Buf(pv.ap.rearrange("p (a b) -> p a b", b=128), pv.lo, pv.hi, 'ps')
                        for hh in range(2):
                            tcopy("dve", vb[g][hh][:, t4 * 4:(t4 + 1) * 4, 0:64], pv3[:, :, hh * 64:hh * 64 + 64])
                calls = []

                def fin_dil(po, hp, hs_, tq):
                    recip(rd[64:128, :], po[64:128, :])
                    tt("dve", ob[hp][hs_, cs(tq)], po[0:64, :], rd[64:128, :], ALU.mult)

                for hh in range(2):
                    hs_ = slice(hh * 64, hh * 64 + 64)
                    for tq in range(4):
                        steps = []
                        q0 = tq * 512
                        for g in range(3):
                            wn = WIN[g]
                            lo_kt = max(0, (512 * tq - wn) // 128)
                            kts = list(range(lo_kt, 4 * tq + 4))
                            if g == 0:
                                kts = [4 * tq] + [k_ for k_ in kts if k_ != 4 * tq]
                            for kt in kts:
                                dl = 512 * tq - 128 * kt
                                j0 = max(0, -dl)
                                j1 = min(512, wn + 128 - dl)
                                if j1 <= j0:
                                    continue
                                dlm = min(dl, 128) if g == 2 else dl
                                masks = [(ident_bf, WTAB[g][:, dlm + 384 + j0:dlm + 384 + j1])]
                                steps.append(dict(j0=j0, j1=j1, k=kb[g][hs_, kt * 128:(kt + 1) * 128],
                                                  q=qb[g][hs_, q0 + j0:q0 + j1], v=vb[g][hh][:, kt, :], masks=masks))
                        calls.append((steps, lambda po, hp=hp, hs_=hs_, tq=tq: fin_dil(po, hp, hs_, tq)))
                attn_run(calls, ptiles, pidx)

            if stop_after == "m_dil":
                top[0] = m_layer
                return
            top[0] = m_attn
            pa = alloc([128, 4, D], BF16)
            pb = alloc([128, 2, D], BF16)
            yT = alloc([128, 8, S], BF16)
            g0 = alloc([128, 512], F32)
            g1 = alloc([128, 512], F32)
            u1 = alloc([128, 512], F32)
            u2 = alloc([128, 512], F32)
            wst = [alloc([128, 8, 128], BF16) for _ in range(4)]
            wload(pa, dr["pa%d" % l].rearrange("(c p) n -> p c n", p=128))
            wload(pb, dr["pb%d" % l].rearrange("(c p) n -> p c n", p=128))
            for f in range(8):
                fs_ = slice(f * 128, (f + 1) * 128)
                wm0 = next_w()
                wm1 = next_w()
                wload(wm0, w_in[:, :, 3608 + f * 128:3608 + (f + 1) * 128])
                wload(wm1, w_in[:, :, 4632 + f * 128:4632 + (f + 1) * 128])
                for tq in range(4):
                    pA = ps_next("O")
                    for c in range(4):
                        mm(pA, pa[:, c, fs_], oa[c][:, cs(tq)], c == 0, c == 3)
                    pB = ps_next("O")
                    for c in range(2):
                        mm(pB, pb[:, c, fs_], ob[c][:, cs(tq)], c == 0, c == 1)
                    pC = ps_next("S")
                    proj_fm(wm0, slice(0, 128), tq, pC)
                    pD = ps_next("S")
                    proj_fm(wm1, slice(0, 128), tq, pD)
                    act(g0, pC, AF.Sigmoid)
                    act(g1, pD, AF.Sigmoid)
                    tt("dve", u1, g0, pA, ALU.mult)
                    tt("dve", u2, g1, pB, ALU.mult)
                    tt("pool", yT[:, f, cs(tq)], u1, u2, ALU.add)
            top[0] = m_attn
            pa_ = alloc([128, 4, D], BF16)
            pb_ = alloc([128, 2, D], BF16)
            yT = alloc([128, 8, S], BF16)
            wo = alloc([128, 8, D], BF16)
            wload(wo, dr["wo%d" % l].rearrange("(c p) n -> p c n", p=128))
            for fo in range(8):
                for tq in range(4):
                    pO = ps_next("O")
                    for f in range(8):
                        mm(pO, wo[:, f, fo * 128:(fo + 1) * 128], yT[:, f, cs(tq)], f == 0, f == 7)
                    tt("dve", xT[:, fo, cs(tq)], xT[:, fo, cs(tq)], pO, ALU.add)
            top[0] = m_layer

        def ffn(l):
            m0 = top[0]
            moe = (l == 1)
            rstdF = [alloc([128, 512], F32) for _ in range(4)]
            rmsnorm(2 * l + 1, rstd_keep=rstdF)
            uT = alloc([128, NJ, 1024], BF16)
            w13 = [alloc([128, 8, 256], BF16) for _ in range(4)]
            w2b = [alloc([128, NJ, 256], BF16) for _ in range(2)]
            sg_ = [alloc([128, 512], BF16) for _ in range(2)]
            tf = [alloc([128, 512], F32) for _ in range(2)]
            cbt = [alloc([128, 512], F32) for _ in range(2)]
            wi = [0, 0]
            if moe:
                comb = alloc([128, 16, 8], F32)
                rg = alloc([128, 8, 8], F32)
                lg = alloc([128, 16], F32)
                lgs = alloc([128, 8], F32)
                m8 = alloc([128, 8], F32)
                sm = alloc([128, 8], F32)
                c0 = alloc([128, 8], F32)
                c1 = alloc([128, 8], F32)
                dgs = [alloc([128, 128], F32) for _ in range(2)]
                dgh = [alloc([128, 128], BF16) for _ in range(2)]
                dgl = [alloc([128, 128], BF16) for _ in range(2)]
                dma("sp", rg, dr["router"].rearrange("(c p) e -> p c e", p=128))
                for f in range(8):
                    ts("dve", rg[:, f, :], rg[:, f, :], gains[:, 2 * l + 1, f:f + 1], None, ALU.mult)
                rgh = alloc([128, 8, 8], BF16)
                rgl = alloc([128, 8, 8], BF16)
                tcopy("dve", rgh, rg)
                tt("dve", rgl, rg, rgh, ALU.subtract)
                xh = [alloc([128, 128], BF16) for _ in range(2)]
                xl = [alloc([128, 128], BF16) for _ in range(2)]
                for ti in range(16):
                    tq, tl = ti // 4, ti % 4
                    pr = ps_next("M")
                    for f in range(8):
                        xh_, xl_ = xh[f % 2], xl[f % 2]
                        tcopy("dve", xh_, xT[:, f, ti * 128:(ti + 1) * 128])
                        tt("dve", xl_, xT[:, f, ti * 128:(ti + 1) * 128], xh_, ALU.subtract)
                        mm(pr[:, 0:8], xh_, rgh[:, f, :], f == 0, False)
                        mm(pr[:, 0:8], xh_, rgl[:, f, :], False, False)
                        mm(pr[:, 0:8], xl_, rgh[:, f, :], False, f == 7)
                    xh_, xl_ = xh[0], xl[0]
                    tcopy("dve", xh_, rstdF[tq][:, tl * 128:(tl + 1) * 128])
                    tt("dve", xl_, rstdF[tq][:, tl * 128:(tl + 1) * 128], xh_, ALU.subtract)
                    mm(pr[:, 8:9], xh_, ident_bf[:, 0:1], False, False)
                    mm(pr[:, 8:9], xl_, ident_bf[:, 0:1], False, True)
                    tcopy("dve", lg[:, 0:9], pr[:, 0:9])
                    ts("dve", lgs, lg[:, 0:8], lg[:, 8:9], None, ALU.mult)
                    P.op("dve", lambda e: e.max(out=m8.ap, in_=lgs.ap), reads=[lgs], writes=[m8])
                    tt("dve", sm[:, 0:1], m8[:, 1:2], m8[:, 0:1], ALU.subtract)
                    act(sm[:, 1:2], sm[:, 0:1], AF.Exp)
                    ts("dve", sm[:, 2:3], sm[:, 1:2], 1.0, None, ALU.add)
                    recip(sm[:, 3:4], sm[:, 2:3])
                    tt("dve", sm[:, 4:5], sm[:, 1:2], sm[:, 3:4], ALU.mult)
                    ts("dve", c0, lgs, m8[:, 0:1], None, ALU.is_equal)
                    ts("dve", c0, c0, sm[:, 3:4], None, ALU.mult)
                    ts("dve", c1, lgs, m8[:, 1:2], None, ALU.is_equal)
                    ts("dve", c1, c1, sm[:, 4:5], None, ALU.mult)
                    tt("dve", comb[:, ti, :], c0, c1, ALU.add)
            nexp = 8 if moe else 1
            items = []
            for th in range(2):
                for e_ in range(nexp):
                    if moe:
                        W1 = dr["m_w1"][e_].rearrange("(c p) n -> p c n", p=128)
                        W3 = dr["m_w3"][e_].rearrange("(c p) n -> p c n", p=128)
                        W2 = dr["m_w2"][e_].rearrange("(j p) n -> p j n", p=128)
                    else:
                        W1 = dr["f_w1"][0].rearrange("(c p) n -> p c n", p=128)
                        W3 = dr["f_w3"][0].rearrange("(c p) n -> p c n", p=128)
                        W2 = dr["f_w2"][0].rearrange("(j p) n -> p j n", p=128)
                    for jp in range(NJ // 2):
                        items.append(("w13", th, e_, jp, W1, W3, W2))
                    for fq in range(4):
                        items.append(("w2", th, e_, fq, W1, W3, W2))

            def do_load(it):
                kind, th, e_, idx, W1, W3, W2 = it
                if kind == "w13":
                    wa = w13[wi[0] % 4]
                    wb = w13[(wi[0] + 1) % 4]
                    wi[0] += 2
                    wload(wa, W1[:, :, idx * 256:(idx + 1) * 256])
                    wload(wb, W3[:, :, idx * 256:(idx + 1) * 256])
                    return (wa, wb)
                else:
                    w2 = w2b[wi[1] % 2]
                    wi[1] += 1
                    wload(w2, W2[:, :, idx * 256:(idx + 1) * 256])
                    return (w2,)

            def do_compute(it, bufs):
                kind, th, e_, idx, W1, W3, W2 = it
                if kind == "w13":
                    wa, wb = bufs
                    jp = idx
                    if moe and jp == 0:
                        for t2_ in range(2):
                            tq = th * 2 + t2_
                            pc = ps_next("M")
                            for tl in range(4):
                                ti = tq * 4 + tl
                                dg = dgs[tl % 2]
                                ts("dve", dg, ident_f, comb[:, ti, e_:e_ + 1], None, ALU.mult)
                                dgh_, dgl_ = dgh[tl % 2], dgl[tl % 2]
                                tcopy("dve", dgh_, dg)
                                tt("dve", dgl_, dg, dgh_, ALU.subtract)
                                mm(pc[:, tl * 128:(tl + 1) * 128], dr_ones1, dgh_, True, False)
                                mm(pc[:, tl * 128:(tl + 1) * 128], dr_ones1, dgl_, False, True)
                            tcopy("dve", cbt[t2_], pc)
                    for jj in range(2):
                        j = jp * 2 + jj
                        for t2_ in range(2):
                            tq = th * 2 + t2_
                            pA = ps_next("O")
                            for d in range(8):
                                mm(pA, wa[:, d, jj * 128:(jj + 1) * 128], hT[:, d, cs(tq)], d == 0, d == 7)
                            pB = ps_next("O")
                            for d in range(8):
                                mm(pB, wb[:, d, jj * 128:(jj + 1) * 128], hT[:, d, cs(tq)], d == 0, d == 7)
                            s_ = sg_[(j * 2 + t2_) % 2]
                            act(s_, pA, AF.Silu)
                            if moe:
                                t_ = tf[(j * 2 + t2_) % 2]
                                tt("dve", t_, s_, pB, ALU.mult)
                                tt("pool", uT[:, j, t2_ * 512:(t2_ + 1) * 512], t_, cbt[t2_], ALU.mult)
                            else:
                                tt("dve", uT[:, j, t2_ * 512:(t2_ + 1) * 512], s_, pB, ALU.mult)
                else:
                    (w2,) = bufs
                    fq = idx
                    for f2 in range(2):
                        fo = fq * 2 + f2
                        for t2_ in range(2):
                            tq = th * 2 + t2_
                            pO = ps_next("M")
                            for j in range(NJ):
                                mm(pO, w2[:, j, f2 * 128:(f2 + 1) * 128], uT[:, j, t2_ * 512:(t2_ + 1) * 512], j == 0, j == NJ - 1)
                            tt("dve", xT[:, fo, cs(tq)], xT[:, fo, cs(tq)], pO, ALU.add)

            loaded = {}
            loaded[0] = do_load(items[0])
            for i, it in enumerate(items):
                if i + 1 < len(items):
                    loaded[i + 1] = do_load(items[i + 1])
                do_compute(it, loaded.pop(i))
            top[0] = m0

        ones1 = alloc([128, 128], BF16)
        dma("sp", ones1, dr["c_ones1_f"])
        dr_ones1 = ones1
        PH0 = top[0]

        stages = ["mixer0", "ffn0", "mixer1", "ffn1", "full"]
        si = stages.index(stop_after) if stop_after in stages else 0
        try:
            mixer(0)
        except StopBuild:
            pass
        if si >= 1:
            ffn(0)
        if si >= 2:
            mixer(1)
        if si >= 3:
            ffn(1)
        outs = []
        if si >= 4:
            sq = [alloc([128, 512], F32) for _ in range(2)]
            sqh = [alloc([128, 512], BF16) for _ in range(2)]
            sql = [alloc([128, 512], BF16) for _ in range(2)]
            rstd = [alloc([128, 512], F32) for _ in range(2)]
            for tq in range(4):
                pm = ps_next("M")
                for f in range(8):
                    s_ = sq[f % 2]
                    act(s_, xT[:, f, cs(tq)], AF.Square)
                    tcopy("dve", sqh[f % 2], s_)
                    tt("dve", sql[f % 2], s_, sqh[f % 2], ALU.subtract)
                    mm(pm, ones_f, sqh[f % 2], f == 0, False)
                    mm(pm, ones_f, sql[f % 2], False, f == 7)
                r_ = rstd[tq % 2]
                ts("dve", r_, pm, EPS, None, ALU.add)
                act(r_, r_, AF.Sqrt)
                recip(r_, r_)
                for f in range(8):
                    stt("dve", xT[:, f, cs(tq)], xT[:, f, cs(tq)], gains[:, 4, f:f + 1], r_, ALU.mult, ALU.mult)
        for f in range(8):
            o = P.op("sp", lambda e, f=f: e.dma_start(out=outT[f * 128:(f + 1) * 128, :], in_=xT.ap[:, f, :]),
                     reads=[xT[:, f, :]], dma=True)
            outs.append(o)
        fin = P.op("sp", lambda e: e.nop())
        for o in outs:
            fin.deps.add(o.id)
        P.finalize(sems, dsems, block)
    nc._declared = set(dr.keys())
    return nc, P


_CACHE = {}


def make_in_maps(inputs):
    hc = host_consts()
    x = np.asarray(inputs["x"], np.float32)
    pos = np.asarray(inputs["positions"], np.int32)
    shared = dict(hc)
    for l in range(2):
        shared["w_in%d" % l] = np.ascontiguousarray(inputs["w_in"][l], dtype=np.float32)
        shared["g_mix%d" % l] = np.ascontiguousarray(inputs["norm_mix"][l], dtype=np.float32)
        shared["g_ffn%d" % l] = np.ascontiguousarray(inputs["norm_ffn"][l], dtype=np.float32)
        shared["posk%d" % l] = np.ascontiguousarray(inputs["cmp_pos_k"][l], dtype=np.float32)
        shared["posv%d" % l] = np.ascontiguousarray(inputs["cmp_pos_v"][l], dtype=np.float32)
        shared["kw1_%d" % l] = np.ascontiguousarray(inputs["cmp_k_w1"][l], dtype=np.float32)
        shared["kw2_%d" % l] = np.ascontiguousarray(inputs["cmp_k_w2"][l], dtype=np.float32)
        shared["vw1_%d" % l] = np.ascontiguousarray(inputs["cmp_v_w1"][l], dtype=np.float32)
        shared["vw2_%d" % l] = np.ascontiguousarray(inputs["cmp_v_w2"][l], dtype=np.float32)
        shared["pa%d" % l] = np.ascontiguousarray(inputs["w_branch_a"][l], dtype=np.float32)
        shared["pb%d" % l] = np.ascontiguousarray(inputs["w_branch_b"][l], dtype=np.float32)
        shared["wo%d" % l] = np.ascontiguousarray(inputs["w_out"][l], dtype=np.float32)
    shared["f_w1"] = np.ascontiguousarray(inputs["ffn_w1"], dtype=np.float32)
    shared["f_w3"] = np.ascontiguousarray(inputs["ffn_w3"], dtype=np.float32)
    shared["f_w2"] = np.ascontiguousarray(inputs["ffn_w2"], dtype=np.float32)
    shared["router"] = np.ascontiguousarray(inputs["router"][0], dtype=np.float32)
    shared["m_w1"] = np.ascontiguousarray(inputs["moe_w1"][0], dtype=np.float32)
    shared["m_w3"] = np.ascontiguousarray(inputs["moe_w3"][0], dtype=np.float32)
    shared["m_w2"] = np.ascontiguousarray(inputs["moe_w2"][0], dtype=np.float32)
    shared["g_fin"] = np.ascontiguousarray(inputs["final_norm"], dtype=np.float32)
    maps = []
    for b in range(8):
        m = dict(shared)
        m["xT"] = np.ascontiguousarray(x[b].T)
        m["pos"] = np.ascontiguousarray(pos[b:b + 1])
        maps.append(m)
    return maps


def kernel(stop_after="full", **inputs):
    if stop_after not in _CACHE:
        _CACHE[stop_after] = build_program(stop_after)[0]
    nc = _CACHE[stop_after]
    maps = make_in_maps(inputs)
    decl = nc._declared
    maps = [{k: v for k, v in m.items() if k in decl} for m in maps]
    res = run_bass_kernel_spmd(nc, maps, core_ids=list(range(8)))
    out = np.stack([np.ascontiguousarray(r["outT"].T) for r in res.results], axis=0)
    return out.astype(np.float32)
```

```python
import numpy as np
from contextlib import ExitStack
import concourse.bass as bass
import concourse.mybir as mybir
from concourse.bass_utils import run_bass_kernel_spmd

F32 = mybir.dt.float32
BF16 = mybir.dt.bfloat16
I32 = mybir.dt.int32
AF = mybir.ActivationFunctionType
ALU = mybir.AluOpType

GRAN = 256
N_DMA_SEM = 12
NEG = -30000.0
S = 2048
D = 1024
DFF = 2816
NJ = DFF // 128
IN_COLS = 5656
EPS = 1e-6


class Buf:
    def __init__(self, ap, lo, hi, space, shape=None, esz=2, exact=True):
        self.ap = ap
        self.lo = lo
        self.hi = hi
        self.space = space
        self.shape = shape
        self.esz = esz
        self.exact = exact and shape is not None

    def grans(self):
        if self.space == 'ps':
            return range((1 << 20) + self.lo // 2048, (1 << 20) + (self.hi - 1) // 2048 + 1)
        return range(self.lo // GRAN, (self.hi - 1) // GRAN + 1)

    def __getitem__(self, key):
        if not isinstance(key, tuple):
            key = (key,)
        ap = self.ap[key]
        if not self.exact:
            return Buf(ap, self.lo, self.hi, self.space, None, self.esz, False)
        key = key + (slice(None),) * (len(self.shape) - len(key))
        fs = self.shape[1:]
        strides = [1] * len(fs)
        for i in range(len(fs) - 2, -1, -1):
            strides[i] = strides[i + 1] * fs[i + 1]
        mn = mx = 0
        for i, k in enumerate(key[1:]):
            n = fs[i]
            if isinstance(k, int):
                a = b = k
            else:
                a, b, st = k.indices(n)
                b = a + ((b - a - 1) // st) * st
            mn += a * strides[i]
            mx += b * strides[i]
        return Buf(ap, self.lo + mn * self.esz, self.lo + (mx + 1) * self.esz, self.space, None, self.esz, False)


class Op:
    __slots__ = ("id", "eng", "emit", "deps", "is_dma", "val", "needed", "qeng")


class Prog:
    ENGS = ("pe", "act", "dve", "pool", "sp")

    def __init__(self):
        self.ops = []
        self.lastw = {}
        self.lastr = {}
        self.dma_rr = {"sp": 0, "pool": 0}
        self.dma_last = {"sp": [None] * N_DMA_SEM, "pool": [None] * N_DMA_SEM}

    def op(self, eng, emit, reads=(), writes=(), dma=False):
        o = Op()
        o.id = len(self.ops)
        o.emit = emit
        o.is_dma = dma
        o.qeng = eng
        o.needed = False
        o.val = None
        deps = set()
        if dma:
            k = self.dma_rr[eng]
            self.dma_rr[eng] = (k + 1) % N_DMA_SEM
            o.eng = ("d%d" if eng == "sp" else "g%d") % k
            if self.dma_last[eng][k] is not None:
                deps.add(self.dma_last[eng][k])
            self.dma_last[eng][k] = o.id
        else:
            o.eng = eng
        lw, lr = self.lastw, self.lastr
        for b in reads:
            for g in b.grans():
                w = lw.get(g)
                if w is not None:
                    deps.add(w)
        for b in writes:
            for g in b.grans():
                w = lw.get(g)
                if w is not None:
                    deps.add(w)
                rs = lr.get(g)
                if rs:
                    deps.update(rs.values())
        for b in reads:
            for g in b.grans():
                d = lr.get(g)
                if d is None:
                    lr[g] = {o.eng: o.id}
                else:
                    d[o.eng] = o.id
        for b in writes:
            for g in b.grans():
                lw[g] = o.id
                lr[g] = None
        deps.discard(o.id)
        o.deps = deps
        self.ops.append(o)
        return o

    def finalize(self, sems, dsems, block):
        ops = self.ops
        for o in ops:
            nd = set()
            for d in o.deps:
                do = ops[d]
                if do.eng == o.eng and o.eng == "pe":
                    continue
                nd.add(d)
                do.needed = True
            o.deps = nd
        cnt = {}
        for o in ops:
            if o.is_dma:
                o.needed = True
            if o.needed:
                step = 16 if o.is_dma else 1
                cnt[o.eng] = cnt.get(o.eng, 0) + step
                o.val = cnt[o.eng]
        self.final_counts = cnt
        queues = {e: [] for e in self.ENGS}
        for o in ops:
            queues[o.qeng].append(o)

        def semof(k):
            if k[0] == "d" and k[1:].isdigit():
                return dsems[int(k[1:])]
            if k[0] == "g" and k[1:].isdigit():
                return dsems[N_DMA_SEM + int(k[1:])]
            return sems[k]

        def run_queue(qname, engobj):
            waited = {}
            for o in queues[qname]:
                need = {}
                for d in o.deps:
                    do = ops[d]
                    if do.val > need.get(do.eng, 0):
                        need[do.eng] = do.val
                for ek, v in need.items():
                    if waited.get(ek, 0) >= v:
                        continue
                    engobj.wait_ge(semof(ek), v)
                    waited[ek] = v
                ins = o.emit(engobj)
                if o.needed:
                    ins.then_inc(semof(o.eng), 16 if o.is_dma else 1)

        @block.tensor
        def _(e):
            run_queue("pe", e)

        @block.scalar
        def _(e):
            run_queue("act", e)

        @block.vector
        def _(e):
            run_queue("dve", e)

        @block.gpsimd
        def _(e):
            run_queue("pool", e)

        @block.sync
        def _(e):
            run_queue("sp", e)


def host_consts():
    import ml_dtypes
    bf = ml_dtypes.bfloat16
    c = {}
    c["c_ident_bf"] = np.eye(128, dtype=np.float32).astype(bf)
    c["c_ident_f"] = np.eye(128, dtype=np.float32)
    c["c_ones_f"] = np.full((128, 128), 1.0 / D, dtype=np.float32).astype(bf)
    c["c_ones1_f"] = np.ones((128, 128), dtype=np.float32).astype(bf)
    rm = np.zeros((128, 128), np.float32)
    for m in range(128):
        r = m % 64
        if r < 8:
            rm[m + 8, m] = -1.0
        elif r < 16:
            rm[m - 8, m] = 1.0
    c["c_rmat"] = rm.astype(bf)
    inv = (500000.0 ** (-np.arange(0, 16, 2, dtype=np.float32) / 16.0)).astype(np.float32)
    invrow = np.zeros((128, 1), np.float32)
    for p in range(128):
        if p % 64 < 16:
            invrow[p, 0] = inv[p % 8]
    c["c_invrow"] = invrow
    k = np.arange(128)[:, None]

    def wtab(width, f):
        r = np.arange(width)[None, :]
        dl = r - 384 - k
        return np.where(f(dl), 0.0, NEG).astype(np.float32).astype(bf)

    c["c_wwin"] = wtab(1408, lambda d: (d >= 0) & (d <= 511))
    c["c_wcaus"] = wtab(896, lambda d: d >= 0)
    c["c_wg1"] = wtab(1024, lambda d: (d >= 0) & (d <= 128))
    c["c_wg2"] = wtab(1408, lambda d: (d >= 0) & (d <= 512) & (d % 4 == 0))
    c["c_wg3"] = wtab(1024, lambda d: (d >= 0) & (d % 16 == 0))
    t = np.arange(S)[None, :]
    cm = np.where((16 * k + 31 <= t) & (k < 127), 0.0, NEG).astype(np.float32)
    c["c_cmpmask"] = cm.astype(bf)
    E = np.zeros((32, S), np.float32)
    for j in range(32):
        E[j, 64 * j:64 * j + 64] = 1.0
    c["c_E"] = E.astype(bf)
    starts = np.arange(127) * 16
    bstart = np.arange(32) * 64
    ov = np.clip(np.minimum(starts[:, None] + 32, bstart[None, :] + 64) - np.maximum(starts[:, None], bstart[None, :]), 0, None)
    M = np.zeros((128, 33), np.float32)
    M[:127, :32] = ov / 32.0
    M[:, 32] = 1.0
    c["c_maug"] = M.astype(bf)
    fb = np.zeros((128, 16, 32), np.float32)
    for tile in range(16):
        for p in range(128):
            tt = tile * 128 + p
            cur = tt // 64
            for j in range(32):
                if j > cur:
                    v = -1e9
                elif j == 0:
                    v = 3e9
                elif j == cur:
                    v = 2e9
                elif j == cur - 1:
                    v = 1e9
                else:
                    v = 0.0
                fb[p, tile, j] = v
    c["c_forceb"] = fb
    sg = np.zeros((24, 12, 128), np.float32)
    for ch in range(4):
        for br in range(3):
            for m in range(128):
                sg[3 * (2 * ch + m // 64) + br, ch * 3 + br, m] = 1.0
    c["c_selgate"] = sg.astype(bf)
    return c


CONST_SHAPES = None


def build_program(stop_after="full"):
    nc = bass.Bass("TRN2", target_bir_lowering=False)
    P = Prog()
    hc = host_consts()

    def din(name, shape, dt=F32):
        return nc.dram_tensor(name, list(shape), dt, kind="ExternalInput").ap()

    dr = {}
    dr["xT"] = din("xT", [D, S])
    dr["pos"] = din("pos", [1, S], I32)
    for k_, v_ in hc.items():
        dr[k_] = din(k_, v_.shape, F32 if v_.dtype == np.float32 else BF16)
    for l in range(2):
        dr["w_in%d" % l] = din("w_in%d" % l, [D, IN_COLS])
        dr["g_mix%d" % l] = din("g_mix%d" % l, [D])
        dr["g_ffn%d" % l] = din("g_ffn%d" % l, [D])
        dr["posk%d" % l] = din("posk%d" % l, [32, 64])
        dr["posv%d" % l] = din("posv%d" % l, [32, 64])
        dr["kw1_%d" % l] = din("kw1_%d" % l, [2048, 128])
        dr["kw2_%d" % l] = din("kw2_%d" % l, [128, 64])
        dr["vw1_%d" % l] = din("vw1_%d" % l, [2048, 128])
        dr["vw2_%d" % l] = din("vw2_%d" % l, [128, 64])
        dr["pa%d" % l] = din("pa%d" % l, [512, D])
        dr["pb%d" % l] = din("pb%d" % l, [256, D])
        dr["wo%d" % l] = din("wo%d" % l, [D, D])
    _st = ["mixer0", "ffn0", "mixer1", "ffn1", "full"]
    _si = _st.index(stop_after) if stop_after in _st else 0
    if _si >= 1:
        dr["f_w1"] = din("f_w1", [1, D, DFF])
        dr["f_w3"] = din("f_w3", [1, D, DFF])
        dr["f_w2"] = din("f_w2", [1, DFF, D])
    if _si >= 3:
        dr["router"] = din("router", [D, 8])
        dr["m_w1"] = din("m_w1", [8, D, DFF])
        dr["m_w3"] = din("m_w3", [8, D, DFF])
        dr["m_w2"] = din("m_w2", [8, DFF, D])
    dr["g_fin"] = din("g_fin", [D])
    outT = nc.dram_tensor("outT", [D, S], F32, kind="ExternalOutput").ap()

    ARENA_BYTES = 212480
    with ExitStack() as st:
        arena = st.enter_context(nc.sbuf_tensor("arena", [128, ARENA_BYTES // 2], BF16))
        pst = [st.enter_context(nc.psum_tensor("ps%d" % i, [128, 512], F32)) for i in range(8)]
        sems = {e: st.enter_context(nc.semaphore("s_" + e)) for e in Prog.ENGS}
        dsems = [st.enter_context(nc.semaphore("dq%d" % i)) for i in range(2 * N_DMA_SEM)]
        block = st.enter_context(nc.Block())

        top = [0]

        def alloc(shape, dt):
            n = int(np.prod(shape[1:]))
            esz = 2 if dt == BF16 else 4
            nb = n * esz
            lo = (top[0] + 255) // 256 * 256
            assert lo + nb <= ARENA_BYTES, ("SBUF overflow", lo + nb)
            top[0] = lo + nb
            v = arena[:, lo // 2:(lo + nb) // 2]
            if dt != BF16:
                v = v.bitcast(dt)
            if len(shape) == 3:
                v = v.rearrange("p (a b) -> p a b", b=shape[2])
            elif len(shape) == 4:
                v = v.rearrange("p (a b c) -> p a b c", b=shape[2], c=shape[3])
            v = v[0:shape[0]]
            return Buf(v, lo, lo + nb, 'sb', tuple(shape), esz)

        PS = [Buf(pst[i][:], i * 2048, (i + 1) * 2048, 'ps', (128, 512), 4) for i in range(8)]
        rr = {"S": 0, "O": 0, "M": 0}

        def ps_next(kind):
            banks = {"S": (0, 1, 2), "O": (3, 4, 5), "M": (6, 7)}[kind]
            i = rr[kind]
            rr[kind] = (i + 1) % len(banks)
            return PS[banks[i]]

        def bufs_of(*xs):
            return [x for x in xs if isinstance(x, Buf)]

        def sc(x):
            return x.ap if isinstance(x, Buf) else x

        def mm(out, lhsT, rhs, start, stop):
            P.op("pe", lambda e: e.matmul(out.ap, lhsT=lhsT.ap, rhs=rhs.ap, start=start, stop=stop,
                                          skip_group_check=True), reads=[lhsT, rhs], writes=[out])

        def act(out, in_, func, scale=1.0, bias=None):
            if bias is None:
                P.op("act", lambda e: e.activation(out=out.ap, in_=in_.ap, func=func, scale=scale),
                     reads=[in_], writes=[out])
            else:
                P.op("act", lambda e: e.activation(out=out.ap, in_=in_.ap, func=func, scale=scale, bias=bias.ap),
                     reads=[in_, bias], writes=[out])

        def tcopy(eng, out, in_):
            P.op(eng, lambda e: e.tensor_copy(out=out.ap, in_=in_.ap), reads=[in_], writes=[out])

        def tt(eng, out, a, b, op):
            P.op(eng, lambda e: e.tensor_tensor(out=out.ap, in0=a.ap, in1=b.ap, op=op), reads=[a, b], writes=[out])

        def ts(eng, out, a, s1, s2, op0, op1=None):
            if op1 is None:
                P.op(eng, lambda e: e.tensor_scalar(out=out.ap, in0=a.ap, scalar1=sc(s1), scalar2=None, op0=op0),
                     reads=[a] + bufs_of(s1), writes=[out])
            else:
                P.op(eng, lambda e: e.tensor_scalar(out=out.ap, in0=a.ap, scalar1=sc(s1), scalar2=sc(s2), op0=op0, op1=op1),
                     reads=[a] + bufs_of(s1, s2), writes=[out])

        def stt(eng, out, a, s, b, op0, op1):
            P.op(eng, lambda e: e.scalar_tensor_tensor(out=out.ap, in0=a.ap, scalar=sc(s), in1=b.ap, op0=op0, op1=op1),
                 reads=[a, b] + bufs_of(s), writes=[out])

        def recip(out, in_):
            P.op("dve", lambda e: e.reciprocal(out=out.ap, in_=in_.ap), reads=[in_], writes=[out])

        def memset(eng, b, val):
            P.op(eng, lambda e: e.memset(b.ap, val), writes=[b])

        def dma(q, out_buf, in_ap, **kw):
            return P.op(q, lambda e: e.dma_start(out=out_buf.ap, in_=in_ap, **kw), writes=[out_buf], dma=True)

        def wload(out_buf, in_ap, **kw):
            return dma("pool", out_buf, in_ap, **kw)

        xT = alloc([128, 8, S], F32)
        hT = alloc([128, 8, S], BF16)
        ident_bf = alloc([128, 128], BF16)
        ident_f = alloc([128, 128], F32)
        ones_f = alloc([128, 128], BF16)
        rmat = alloc([128, 128], BF16)
        invrow = alloc([128, 1], F32)
        gains = alloc([128, 5, 8], F32)
        posi = None
        dma("sp", ident_bf, dr["c_ident_bf"])
        dma("sp", ident_f, dr["c_ident_f"])
        dma("sp", ones_f, dr["c_ones_f"])
        dma("sp", rmat, dr["c_rmat"])
        dma("sp", invrow, dr["c_invrow"])
        for i, nm in enumerate(["g_mix0", "g_ffn0", "g_mix1", "g_ffn1", "g_fin"]):
            dma("sp", gains[:, i, :], dr[nm].rearrange("(c p) -> p c", p=128), allow_slow_non_contiguous=True)
        for f in range(8):
            dma("sp", xT[:, f, :], dr["xT"][f * 128:(f + 1) * 128, :])
        PH0 = top[0]

        def cs(tq):
            return slice(tq * 512, (tq + 1) * 512)

        def rmsnorm(gain_idx, rstd_keep=None):
            m0 = top[0]
            sq = [alloc([128, 512], F32) for _ in range(2)]
            sqh = [alloc([128, 512], BF16) for _ in range(2)]
            sql = [alloc([128, 512], BF16) for _ in range(2)]
            rs_tmp = [alloc([128, 512], F32) for _ in range(2)]
            for tq in range(4):
                pm = ps_next("M")
                for f in range(8):
                    s_ = sq[f % 2]
                    act(s_, xT[:, f, cs(tq)], AF.Square)
                    tcopy("dve", sqh[f % 2], s_)
                    tt("dve", sql[f % 2], s_, sqh[f % 2], ALU.subtract)
                    mm(pm, ones_f, sqh[f % 2], f == 0, False)
                    mm(pm, ones_f, sql[f % 2], False, f == 7)
                rstd = rstd_keep[tq] if rstd_keep is not None else rs_tmp[tq % 2]
                ts("dve", rstd, pm, EPS, None, ALU.add)
                act(rstd, rstd, AF.Sqrt)
                recip(rstd, rstd)
                for f in range(8):
                    stt("dve", hT[:, f, cs(tq)], xT[:, f, cs(tq)], gains[:, gain_idx, f:f + 1], rstd, ALU.mult, ALU.mult)
            top[0] = m0

        def proj_fm(wt, wsl, tq, ps):
            for d in range(8):
                mm(ps, wt[:, d, wsl], hT[:, d, cs(tq)], d == 0, d == 7)

        def rope_evac(ps, dst, cosT, sinT, tq, tmps):
            import os as _os
            lvl = int(_os.environ.get("DBG_ROPE", "5"))
            raw, t1, t2 = tmps
            if lvl >= 1:
                tcopy("dve", raw, ps)
            if lvl >= 2:
                pp = ps_next("M")
                mm(pp, rmat, raw, True, True)
            if lvl >= 3:
                tt("dve", t1, ps, cosT[:, cs(tq)], ALU.mult)
            if lvl >= 4:
                tt("dve", t2, pp, sinT[:, cs(tq)], ALU.mult)
            if lvl >= 5:
                tt("dve", dst, t1, t2, ALU.add)
            else:
                tcopy("dve", dst, ps)

        def attn_run(calls, ptiles, pidx):
            flat = []
            for ci, (steps, fin) in enumerate(calls):
                fi = [i for i, s_ in enumerate(steps) if s_["j0"] == 0 and s_["j1"] == 512][0]
                steps = [steps[fi]] + steps[:fi] + steps[fi + 1:]
                for si, stp in enumerate(steps):
                    flat.append((ci, si, len(steps), stp, fin))

            def emit_qk(stp):
                j0, j1 = stp["j0"], stp["j1"]
                pS = ps_next("S")
                ml = stp["masks"]
                mm(pS[:, j0:j1], stp["k"], stp["q"], True, len(ml) == 0)
                for mi, mk_ in enumerate(ml):
                    a_, b_ = mk_[0], mk_[1]
                    m0_, m1_ = (mk_[2], mk_[3]) if len(mk_) == 4 else (j0, j1)
                    mm(pS[:, m0_:m1_], a_, b_, False, mi == len(ml) - 1)
                return pS

            LOOK = 2
            pos = {}
            pend = []
            nq = 0
            while nq < min(LOOK, len(flat)):
                pend.append(emit_qk(flat[nq][3]))
                nq += 1
            for gi, (ci, si, n, stp, fin) in enumerate(flat):
                pS = pend.pop(0)
                if nq < len(flat):
                    pend.append(emit_qk(flat[nq][3]))
                    nq += 1
                if si == 0:
                    pos[ci] = ps_next("O")
                po = pos[ci]
                j0, j1 = stp["j0"], stp["j1"]
                pt = ptiles[pidx[0] % len(ptiles)]
                pidx[0] += 1
                act(pt[:, j0:j1], pS[:, j0:j1], AF.Exp, scale=0.125)
                mm(po[:, j0:j1], stp["v"], pt[:, j0:j1], si == 0, si == n - 1)
                if si == n - 1:
                    fin(po)

        class StopBuild(Exception):
            pass

        def chk(name):
            if stop_after == name:
                raise StopBuild()

        def mixer(l):
            m_layer = top[0]
            w_in = dr["w_in%d" % l].rearrange("(c p) n -> p c n", p=128)
            cosT = alloc([128, S], F32)
            sinT = alloc([128, S], F32)
            m1 = top[0]
            posi_ = alloc([128, S], I32)
            ang = alloc([128, S], F32)
            wv_ = alloc([128, S], F32)
            mk_ = alloc([128, S], F32)
            dma("sp", posi_, dr["pos"].partition_broadcast(128))
            tcopy("dve", ang, posi_)
            ts("dve", ang, ang, invrow, float(1.0 / (2 * np.pi)), ALU.mult, ALU.mult)
            for (dst, shift) in ((sinT, 0.0), (cosT, 0.25)):
                if shift != 0.0:
                    ts("dve", ang, ang, shift, None, ALU.add)
                tcopy("dve", posi_, ang)
                tcopy("dve", wv_, posi_)
                tt("dve", wv_, ang, wv_, ALU.subtract)
                ts("dve", mk_, wv_, 0.5, None, ALU.is_gt)
                tt("dve", wv_, wv_, mk_, ALU.subtract)
                ts("dve", mk_, wv_, -0.5, None, ALU.is_lt)
                tt("dve", wv_, wv_, mk_, ALU.add)
                act(dst, wv_, AF.Sin, scale=6.28318)
            top[0] = m1

            rmsnorm(2 * l)
            if stop_after == "m_pre":
                top[0] = m_layer
                return

            oa = [alloc([128, S], BF16) for _ in range(4)]
            ob = [alloc([128, S], BF16) for _ in range(2)]
            m_attn = top[0]

            wwin = alloc([128, 1408], BF16)
            wcaus = alloc([128, 896], BF16)
            cmpmask = alloc([128, S], BF16)
            Emat = alloc([32, S], BF16)
            maug = alloc([128, 33], BF16)
            forceb = alloc([128, 16, 32], F32)
            selgate = alloc([24, 12, 128], BF16)
            for b_, nm in ((wwin, "c_wwin"), (wcaus, "c_wcaus"), (cmpmask, "c_cmpmask"), (Emat, "c_E"),
                           (maug, "c_maug"), (forceb, "c_forceb"), (selgate, "c_selgate")):
                dma("sp", b_, dr[nm])
            GT = alloc([24, S], BF16)
            ptiles = [alloc([128, 512], BF16) for _ in range(3)]
            raw = alloc([128, 512], BF16)
            t1 = alloc([128, 512], F32)
            t2 = alloc([128, 512], F32)
            rtmps = (raw, t1, t2)
            rd = t1
            on = t2
            wst = [alloc([128, 8, 128], BF16) for _ in range(2)]
            wsi = [0]

            def next_w():
                w = wst[wsi[0] % len(wst)]
                wsi[0] += 1
                return w

            wg = next_w()
            wload(wg, w_in[:, :, 1280:1408])
            for tq in range(4):
                pg = ps_next("M")
                for d in range(8):
                    mm(pg[0:24, :], wg[:, d, 0:24], hT[:, d, cs(tq)], d == 0, d == 7)
                act(GT[:, cs(tq)], pg[0:24, :], AF.Sigmoid)
            chk("n0")
            kcT2 = [alloc([128, 128], BF16) for _ in range(2)]
            vaug_cmp = [alloc([128, 128], BF16) for _ in range(2)]
            m_grp = top[0]
            kcmpT = alloc([128, S], BF16)
            vcmpT = alloc([128, S], BF16)
            w1k = alloc([128, 32, 128], BF16)
            w1v = alloc([128, 32, 128], BF16)
            w2k = alloc([128, 128], BF16)
            w2v = alloc([128, 64], BF16)
            pkT = alloc([128, 32], BF16)
            pvT = alloc([128, 32], BF16)
            biask = alloc([128, 1], F32)
            biasv = alloc([128, 1], F32)
            hsk = alloc([128, 128], BF16)
            hsv = alloc([128, 128], BF16)
            wk_ = next_w()
            wload(wk_, w_in[:, :, 512:640])
            for tq in range(4):
                pk = ps_next("M")
                proj_fm(wk_, slice(0, 128), tq, pk)
                rope_evac(pk, kcmpT[:, cs(tq)], cosT, sinT, tq, rtmps)
            chk("p1")
            wv = next_w()
            wload(wv, w_in[:, :, 640:768])
            for tq in range(4):
                pv = ps_next("M")
                proj_fm(wv, slice(0, 128), tq, pv)
                tcopy("dve", vcmpT[:, cs(tq)], pv)
            chk("p2")
            for half in range(2):
                sl = slice(half * 64, half * 64 + 64)
                for l4 in range(4):
                    wload(w1k[sl, l4 * 8:(l4 + 1) * 8, :], dr["kw1_%d" % l].rearrange("(l d) h -> d l h", d=64)[:, l4 * 8:(l4 + 1) * 8, :])
                    wload(w1v[sl, l4 * 8:(l4 + 1) * 8, :], dr["vw1_%d" % l].rearrange("(l d) h -> d l h", d=64)[:, l4 * 8:(l4 + 1) * 8, :])
            chk("p3")
            w2st = alloc([128, 128], F32)
            dma("sp", w2st[:, 0:64], dr["kw2_%d" % l])
            dma("sp", w2st[:, 64:128], dr["vw2_%d" % l])
            tcopy("dve", w2k[:, 0:64], w2st[:, 0:64])
            tcopy("dve", w2k[:, 64:128], w2st[:, 0:64])
            tcopy("dve", w2v, w2st[:, 64:128])
            chk("p4")
            posn = alloc([32, 128], F32)
            dma("sp", posn[:, 0:64], dr["posk%d" % l])
            dma("sp", posn[:, 64:128], dr["posv%d" % l])
            posnb = alloc([32, 128], BF16)
            tcopy("dve", posnb, posn)
            for (pxT, c0_) in ((pkT, 0), (pvT, 64)):
                ptp = ps_next("M")
                mm(ptp[0:64, 0:32], posnb[:, c0_:c0_ + 64], ident_bf[0:32, 0:32], True, True)
                tcopy("dve", pxT[0:64, :], ptp[0:64, 0:32])
            chk("n1")
            for (w1x, pxT, bx) in ((w1k, pkT, biask), (w1v, pvT, biasv)):
                pb_ = ps_next("M")
                for li in range(32):
                    mm(pb_[:, 0:1], w1x[0:64, li, :], pxT[0:64, li:li + 1], li == 0, li == 31)
                tcopy("dve", bx, pb_[:, 0:1])
            for g in range(2):
                gs = slice(g * 64, g * 64 + 64)
                memset("pool", kcT2[g], 0.0)
                memset("pool", vaug_cmp[g], 0.0)
                memset("pool", vaug_cmp[g][:, 64:128], 1.0)
                ph = ps_next("M")
                for li in range(32):
                    mm(ph[:, 0:127], w1k[gs, li, :], kcmpT[gs, li:li + 16 * 126 + 1:16], li == 0, li == 31)
                act(hsk[:, 0:127], ph[:, 0:127], AF.Silu, bias=biask)
                pk2 = ps_next("M")
                mm(pk2[:, 0:127], w2k, hsk[:, 0:127], True, True)
                tcopy("dve", kcT2[g][:, 0:127], pk2[:, 0:127])
                ph = ps_next("M")
                for li in range(32):
                    mm(ph[:, 0:127], w1v[gs, li, :], vcmpT[gs, li:li + 16 * 126 + 1:16], li == 0, li == 31)
                act(hsv[:, 0:127], ph[:, 0:127], AF.Silu, bias=biasv)
                pv2 = ps_next("M")
                mm(pv2[0:127, 0:64], hsv[:, 0:127], w2v, True, True)
                tcopy("dve", vaug_cmp[g][0:127, 0:64], pv2[0:127, 0:64])

            chk("n2")
            for g in range(2):
                top[0] = m_grp
                kT = [None] + [alloc([128, S], BF16) for _ in range(2)]
                qT1 = alloc([128, S], BF16)
                vaug_sel = alloc([128, 16, 128], BF16)
                vaug_win = alloc([128, 16, 128], BF16)
                imp = alloc([128, 16, 32], F32)
                negsel = alloc([128, 32], BF16)
                negselT = alloc([32, S], BF16)
                m8 = alloc([128, 16], F32)
                mrep = alloc([128, 32], F32)
                rdn = alloc([128, 4], F32)
                wdup = alloc([128, 8, 128], BF16)
                for bi in (1, 2):
                    col = 512 + bi * 256 + g * 64
                    wl_ = next_w()
                    col0 = 512 + bi * 256
                    wload(wl_, w_in[:, :, col0:col0 + 128])
                    w = wdup
                    tcopy("dve", w[:, :, 0:64], wl_[:, :, g * 64:g * 64 + 64])
                    tcopy("dve", w[:, :, 64:128], wl_[:, :, g * 64:g * 64 + 64])
                    for tq in range(4):
                        pk = ps_next("M")
                        proj_fm(w, slice(0, 128), tq, pk)
                        rope_evac(pk, kT[bi][:, cs(tq)], cosT, sinT, tq, rtmps)
                w = wdup
                wl_ = next_w()
                wload(wl_, w_in[:, :, 896:1024])
                tcopy("dve", w[:, :, 0:64], wl_[:, :, g * 64:g * 64 + 64])
                wl_ = next_w()
                wload(wl_, w_in[:, :, 1152:1280])
                tcopy("dve", w[:, :, 64:128], wl_[:, :, g * 64:g * 64 + 64])
                memset("pool", vaug_sel[:, :, 64:128], 1.0)
                memset("pool", vaug_win[:, :, 64:128], 1.0)
                for t4 in range(4):
                    pv = ps_next("M")
                    for tl in range(4):
                        ti = t4 * 4 + tl
                        for d in range(8):
                            mm(pv[:, tl * 128:(tl + 1) * 128], hT[:, d, ti * 128:(ti + 1) * 128], w[:, d, :], d == 0, d == 7)
                    pv3 = Buf(pv.ap.rearrange("p (a b) -> p a b", b=128), pv.lo, pv.hi, 'ps')
                    tcopy("dve", vaug_sel[:, t4 * 4:(t4 + 1) * 4, 0:64], pv3[:, :, 0:64])
                    tcopy("dve", vaug_win[:, t4 * 4:(t4 + 1) * 4, 0:64], pv3[:, :, 64:128])

                def proj_q(cc):
                    c_ = 2 * g + cc
                    w_ = next_w()
                    wload(w_, w_in[:, :, c_ * 128:(c_ + 1) * 128])
                    for tq_ in range(4):
                        pq = ps_next("M")
                        proj_fm(w_, slice(0, 128), tq_, pq)
                        rope_evac(pq, qT1[:, cs(tq_)], cosT, sinT, tq_, rtmps)
                chk("n3")
                tcopy("pool", imp, forceb)

                def finish(po, c, hh, br, tq, first):
                    hs_ = slice(hh * 64, hh * 64 + 64)
                    ts("dve", rd[64:128, :], po[64:128, :], 1e-30, None, ALU.max)
                    recip(rd[64:128, :], rd[64:128, :])
                    tt("dve", on[hs_, :], po[0:64, :], rd[64:128, :], ALU.mult)
                    pg_ = ps_next("M")
                    mm(pg_, selgate[:, c * 3 + br, :], GT[:, cs(tq)], True, True)
                    if first:
                        tt("dve", oa[c][hs_, cs(tq)], on[hs_, :], pg_[hs_, :], ALU.mult)
                    else:
                        tt("dve", on[hs_, :], on[hs_, :], pg_[hs_, :], ALU.mult)
                        tt("dve", oa[c][hs_, cs(tq)], oa[c][hs_, cs(tq)], on[hs_, :], ALU.add)

                pidx = [0]
                for i in range(4):
                    cc, hh = i // 2, i % 2
                    c = 2 * g + cc
                    hs_ = slice(hh * 64, hh * 64 + 64)
                    if hh == 0:
                        proj_q(cc)
                    for tq in range(4):
                        pS = ps_next("S")
                        mm(pS, kcT2[g][hs_, :], qT1[hs_, cs(tq)], True, False)
                        mm(pS, ident_bf, cmpmask[:, cs(tq)], False, True)
                        pt = ptiles[pidx[0] % 3]
                        pidx[0] += 1
                        act(pt, pS, AF.Exp, scale=0.125)
                        po = ps_next("O")
                        mm(po, vaug_cmp[g], pt, True, True)
                        pI = ps_next("M")
                        for ql in range(4):
                            mm(pI[:, ql * 33:(ql + 1) * 33], pt[:, ql * 128:(ql + 1) * 128], maug, True, True)
                        for ql in range(4):
                            qt_ = tq * 4 + ql
                            ts("dve", rdn[:, ql:ql + 1], pI[:, ql * 33 + 32:ql * 33 + 33], 1e-30, None, ALU.max)
                            recip(rdn[:, ql:ql + 1], rdn[:, ql:ql + 1])
                            stt("dve", imp[:, qt_, :], pI[:, ql * 33:ql * 33 + 32], rdn[:, ql:ql + 1], imp[:, qt_, :], ALU.mult, ALU.add)
                        finish(po, c, hh, 0, tq, True)
                chk("n4")
                for tq in range(4):
                    pT = ps_next("M")
                    for ql in range(4):
                        qt_ = tq * 4 + ql
                        P.op("dve", lambda e, qt_=qt_: e.max(out=m8.ap[:, 0:8], in_=imp.ap[:, qt_, :]), reads=[imp[:, qt_, :]], writes=[m8[:, 0:8]])
                        P.op("dve", lambda e, qt_=qt_: e.match_replace(out=mrep.ap, in_to_replace=m8.ap[:, 0:8], in_values=imp.ap[:, qt_, :], imm_value=-3e9),
                             reads=[imp[:, qt_, :], m8[:, 0:8]], writes=[mrep])
                        P.op("dve", lambda e: e.max(out=m8.ap[:, 8:16], in_=mrep.ap), reads=[mrep], writes=[m8[:, 8:16]])
                        ts("dve", negsel, imp[:, qt_, :], m8[:, 15:16], None, ALU.is_lt)
                        ts("dve", negsel, negsel, NEG, None, ALU.mult)
                        mm(pT[0:32, ql * 128:(ql + 1) * 128], negsel, ident_bf, True, True)
                    tcopy("dve", negselT[:, cs(tq)], pT[0:32, :])
                chk("n5")
                for i in range(4):
                    cc, hh = i // 2, i % 2
                    c = 2 * g + cc
                    hs_ = slice(hh * 64, hh * 64 + 64)
                    if hh == 0:
                        proj_q(cc)
                        calls = []
                    for tq in range(4):
                        steps = []
                        kts = list(range(4 * tq + 4))
                        first = 4 * tq
                        kts = [first] + [k_ for k_ in kts if k_ != first]
                        for kt in kts:
                            dl = 512 * tq - 128 * kt
                            j0 = max(0, -dl)
                            j1 = 512
                            q0 = tq * 512
                            masks = [] if tq < 2 else [(Emat[:, kt * 128:(kt + 1) * 128], negselT[:, q0 + j0:q0 + j1])]
                            if kt >= 4 * tq:
                                masks.append((ident_bf, wcaus[:, dl + 384 + j0:dl + 384 + j0 + 128], j0, j0 + 128))
                            steps.append(dict(j0=j0, j1=j1, k=kT[1][hs_, kt * 128:(kt + 1) * 128],
                                              q=qT1[hs_, q0 + j0:q0 + j1], v=vaug_sel[:, kt, :], masks=masks))
                        calls.append((steps, lambda po, c=c, hh=hh, tq=tq: finish(po, c, hh, 1, tq, False)))
                        steps = []
                        kts = list(range(max(0, 4 * tq - 4), 4 * tq + 4))
                        kts = [first] + [k_ for k_ in kts if k_ != first]
                        for kt in kts:
                            dl = 512 * tq - 128 * kt
                            j0 = max(0, -dl)
                            j1 = min(512, 639 - dl)
                            q0 = tq * 512
                            if dl > 0:
                                m0_, m1_ = max(j0, 512 - dl), j1
                            else:
                                m0_, m1_ = j0, min(j1, j0 + 128)
                            masks = [(ident_bf, wwin[:, dl + 384 + m0_:dl + 384 + m1_], m0_, m1_)]
                            steps.append(dict(j0=j0, j1=j1, k=kT[2][hs_, kt * 128:(kt + 1) * 128],
                                              q=qT1[hs_, q0 + j0:q0 + j1], v=vaug_win[:, kt, :], masks=masks))
                        calls.append((steps, lambda po, c=c, hh=hh, tq=tq: finish(po, c, hh, 2, tq, False)))
                    if hh == 1:
                        attn_run(calls, ptiles, pidx)

            if stop_after == "m_nsa":
                top[0] = m_layer
                return
            top[0] = m_attn
            wg1 = alloc([128, 1024], BF16)
            wg2 = alloc([128, 1408], BF16)
            wg3 = alloc([128, 1024], BF16)
            for b_, nm in ((wg1, "c_wg1"), (wg2, "c_wg2"), (wg3, "c_wg3")):
                dma("sp", b_, dr[nm])
            ptiles = [alloc([128, 512], BF16) for _ in range(3)]
            raw = alloc([128, 512], BF16)
            t1 = alloc([128, 512], F32)
            t2 = alloc([128, 512], F32)
            rtmps = (raw, t1, t2)
            rd = t1
            wst = [alloc([128, 8, 128], BF16) for _ in range(2)]
            wsi = [0]
            qb = [alloc([128, S], BF16) for _ in range(3)]
            kb = [alloc([128, S], BF16) for _ in range(3)]
            vb = [[alloc([128, 16, 128], BF16) for _ in range(2)] for _ in range(3)]
            pidx = [0]
            WTAB = (wg1, wg2, wg3)
            WIN = (128, 512, 2048)
            for hp in range(2):
                for g in range(3):
                    for kind, dstl in ((0, qb), (1, kb)):
                        col = 1304 + kind * 768 + g * 256 + hp * 128
                        w = next_w()
                        wload(w, w_in[:, :, col:col + 128])
                        for tq in range(4):
                            pq = ps_next("M")
                            proj_fm(w, slice(0, 128), tq, pq)
                            rope_evac(pq, dstl[g][:, cs(tq)], cosT, sinT, tq, rtmps)
                    col = 1304 + 2 * 768 + g * 256 + hp * 128
                    w = next_w()
                    wload(w, w_in[:, :, col:col + 128])
                    for hh in range(2):
                        memset("pool", vb[g][hh][:, :, 64:128], 1.0)
                    for t4 in range(4):
                        pv = ps_next("M")
                        for tl in range(4):
                            ti = t4 * 4 + tl
                            for d in range(8):
                                mm(pv[:, tl * 128:(tl + 1) * 128], hT[:, d, ti * 128:(ti + 1) * 128], w[:, d, :], d == 0, d == 7)
                        pv3 = Buf(pv.ap.rearrange("p (a b) -> p a b", b=128), pv.lo, pv.hi, 'ps')
                        for hh in range(2):
                            tcopy("dve", vb[g][hh][:, t4 * 4:(t4 + 1) * 4, 0:64], pv3[:, :, hh * 64:hh * 64 + 64])
                calls = []

                def fin_dil(po, hp, hs_, tq):
                    recip(rd[64:128, :], po[64:128, :])
                    tt("dve", ob[hp][hs_, cs(tq)], po[0:64, :], rd[64:128, :], ALU.mult)

                for hh in range(2):
                    hs_ = slice(hh * 64, hh * 64 + 64)
                    for tq in range(4):
                        steps = []
                        q0 = tq * 512
                        for g in range(3):
                            wn = WIN[g]
                            lo_kt = max(0, (512 * tq - wn) // 128)
                            kts = list(range(lo_kt, 4 * tq + 4))
                            if g == 0:
                                kts = [4 * tq] + [k_ for k_ in kts if k_ != 4 * tq]
                            for kt in kts:
                                dl = 512 * tq - 128 * kt
                                j0 = max(0, -dl)
                                j1 = min(512, wn + 128 - dl)
                                if j1 <= j0:
                                    continue
                                dlm = min(dl, 128) if g == 2 else dl
                                masks = [(ident_bf, WTAB[g][:, dlm + 384 + j0:dlm + 384 + j1])]
                                steps.append(dict(j0=j0, j1=j1, k=kb[g][hs_, kt * 128:(kt + 1) * 128],
                                                  q=qb[g][hs_, q0 + j0:q0 + j1], v=vb[g][hh][:, kt, :], masks=masks))
                        calls.append((steps, lambda po, hp=hp, hs_=hs_, tq=tq: fin_dil(po, hp, hs_, tq)))
                attn_run(calls, ptiles, pidx)

            if stop_after == "m_dil":
                top[0] = m_layer
                return
            top[0] = m_attn
            pa = alloc([128, 4, D], BF16)
            pb = alloc([128, 2, D], BF16)
            yT = alloc([128, 8, S], BF16)
            g0 = alloc([128, 512], F32)
            g1 = alloc([128, 512], F32)
            u1 = alloc([128, 512], F32)
            u2 = alloc([128, 512], F32)
            wst = [alloc([128, 8, 128], BF16) for _ in range(4)]
            wload(pa, dr["pa%d" % l].rearrange("(c p) n -> p c n", p=128))
            wload(pb, dr["pb%d" % l].rearrange("(c p) n -> p c n", p=128))
            for f in range(8):
                fs_ = slice(f * 128, (f + 1) * 128)
                wm0 = next_w()
                wm1 = next_w()
                wload(wm0, w_in[:, :, 3608 + f * 128:3608 + (f + 1) * 128])
                wload(wm1, w_in[:, :, 4632 + f * 128:4632 + (f + 1) * 128])
                for tq in range(4):
                    pA = ps_next("O")
                    for c in range(4):
                        mm(pA, pa[:, c, fs_], oa[c][:, cs(tq)], c == 0, c == 3)
                    pB = ps_next("O")
                    for c in range(2):
                        mm(pB, pb[:, c, fs_], ob[c][:, cs(tq)], c == 0, c == 1)
                    pC = ps_next("S")
                    proj_fm(wm0, slice(0, 128), tq, pC)
                    pD = ps_next("S")
                    proj_fm(wm1, slice(0, 128), tq, pD)
                    act(g0, pC, AF.Sigmoid)
                    act(g1, pD, AF.Sigmoid)
                    tt("dve", u1, g0, pA, ALU.mult)
                    tt("dve", u2, g1, pB, ALU.mult)
                    tt("pool", yT[:, f, cs(tq)], u1, u2, ALU.add)
            top[0] = m_attn
            pa_ = alloc([128, 4, D], BF16)
            pb_ = alloc([128, 2, D], BF16)
            yT = alloc([128, 8, S], BF16)
            wo = alloc([128, 8, D], BF16)
            wload(wo, dr["wo%d" % l].rearrange("(c p) n -> p c n", p=128))
            for fo in range(8):
                for tq in range(4):
                    pO = ps_next("O")
                    for f in range(8):
                        mm(pO, wo[:, f, fo * 128:(fo + 1) * 128], yT[:, f, cs(tq)], f == 0, f == 7)
                    tt("dve", xT[:, fo, cs(tq)], xT[:, fo, cs(tq)], pO, ALU.add)
            top[0] = m_layer

        def ffn(l):
            m0 = top[0]
            moe = (l == 1)
            rstdF = [alloc([128, 512], F32) for _ in range(4)]
            rmsnorm(2 * l + 1, rstd_keep=rstdF)
            uT = alloc([128, NJ, 1024], BF16)
            w13 = [alloc([128, 8, 256], BF16) for _ in range(4)]
            w2b = [alloc([128, NJ, 256], BF16) for _ in range(2)]
            sg_ = [alloc([128, 512], BF16) for _ in range(2)]
            tf = [alloc([128, 512], F32) for _ in range(2)]
            cbt = [alloc([128, 512], F32) for _ in range(2)]
            wi = [0, 0]
            if moe:
                comb = alloc([128, 16, 8], F32)
                rg = alloc([128, 8, 8], F32)
                lg = alloc([128, 16], F32)
                lgs = alloc([128, 8], F32)
                m8 = alloc([128, 8], F32)
                sm = alloc([128, 8], F32)
                c0 = alloc([128, 8], F32)
                c1 = alloc([128, 8], F32)
                dgs = [alloc([128, 128], F32) for _ in range(2)]
                dgh = [alloc([128, 128], BF16) for _ in range(2)]
                dgl = [alloc([128, 128], BF16) for _ in range(2)]
                dma("sp", rg, dr["router"].rearrange("(c p) e -> p c e", p=128))
                for f in range(8):
                    ts("dve", rg[:, f, :], rg[:, f, :], gains[:, 2 * l + 1, f:f + 1], None, ALU.mult)
                rgh = alloc([128, 8, 8], BF16)
                rgl = alloc([128, 8, 8], BF16)
                tcopy("dve", rgh, rg)
                tt("dve", rgl, rg, rgh, ALU.subtract)
                xh = [alloc([128, 128], BF16) for _ in range(2)]
                xl = [alloc([128, 128], BF16) for _ in range(2)]
                for ti in range(16):
                    tq, tl = ti // 4, ti % 4
                    pr = ps_next("M")
                    for f in range(8):
                        xh_, xl_ = xh[f % 2], xl[f % 2]
                        tcopy("dve", xh_, xT[:, f, ti * 128:(ti + 1) * 128])
                        tt("dve", xl_, xT[:, f, ti * 128:(ti + 1) * 128], xh_, ALU.subtract)
                        mm(pr[:, 0:8], xh_, rgh[:, f, :], f == 0, False)
                        mm(pr[:, 0:8], xh_, rgl[:, f, :], False, False)
                        mm(pr[:, 0:8], xl_, rgh[:, f, :], False, f == 7)
                    xh_, xl_ = xh[0], xl[0]
                    tcopy("dve", xh_, rstdF[tq][:, tl * 128:(tl + 1) * 128])
                    tt("dve", xl_, rstdF[tq][:, tl * 128:(tl + 1) * 128], xh_, ALU.subtract)
                    mm(pr[:, 8:9], xh_, ident_bf[:, 0:1], False, False)
                    mm(pr[:, 8:9], xl_, ident_bf[:, 0:1], False, True)
                    tcopy("dve", lg[:, 0:9], pr[:, 0:9])
                    ts("dve", lgs, lg[:, 0:8], lg[:, 8:9], None, ALU.mult)
                    P.op("dve", lambda e: e.max(out=m8.ap, in_=lgs.ap), reads=[lgs], writes=[m8])
                    tt("dve", sm[:, 0:1], m8[:, 1:2], m8[:, 0:1], ALU.subtract)
                    act(sm[:, 1:2], sm[:, 0:1], AF.Exp)
                    ts("dve", sm[:, 2:3], sm[:, 1:2], 1.0, None, ALU.add)
                    recip(sm[:, 3:4], sm[:, 2:3])
                    tt("dve", sm[:, 4:5], sm[:, 1:2], sm[:, 3:4], ALU.mult)
                    ts("dve", c0, lgs, m8[:, 0:1], None, ALU.is_equal)
                    ts("dve", c0, c0, sm[:, 3:4], None, ALU.mult)
                    ts("dve", c1, lgs, m8[:, 1:2], None, ALU.is_equal)
                    ts("dve", c1, c1, sm[:, 4:5], None, ALU.mult)
                    tt("dve", comb[:, ti, :], c0, c1, ALU.add)
            nexp = 8 if moe else 1
            items = []
            for th in range(2):
                for e_ in range(nexp):
                    if moe:
                        W1 = dr["m_w1"][e_].rearrange("(c p) n -> p c n", p=128)
                        W3 = dr["m_w3"][e_].rearrange("(c p) n -> p c n", p=128)
                        W2 = dr["m_w2"][e_].rearrange("(j p) n -> p j n", p=128)
                    else:
                        W1 = dr["f_w1"][0].rearrange("(c p) n -> p c n", p=128)
                        W3 = dr["f_w3"][0].rearrange("(c p) n -> p c n", p=128)
                        W2 = dr["f_w2"][0].rearrange("(j p) n -> p j n", p=128)
                    for jp in range(NJ // 2):
                        items.append(("w13", th, e_, jp, W1, W3, W2))
                    for fq in range(4):
                        items.append(("w2", th, e_, fq, W1, W3, W2))

            def do_load(it):
                kind, th, e_, idx, W1, W3, W2 = it
                if kind == "w13":
                    wa = w13[wi[0] % 4]
                    wb = w13[(wi[0] + 1) % 4]
                    wi[0] += 2
                    wload(wa, W1[:, :, idx * 256:(idx + 1) * 256])
                    wload(wb, W3[:, :, idx * 256:(idx + 1) * 256])
                    return (wa, wb)
                else:
                    w2 = w2b[wi[1] % 2]
                    wi[1] += 1
                    wload(w2, W2[:, :, idx * 256:(idx + 1) * 256])
                    return (w2,)

            def do_compute(it, bufs):
                kind, th, e_, idx, W1, W3, W2 = it
                if kind == "w13":
                    wa, wb = bufs
                    jp = idx
                    if moe and jp == 0:
                        for t2_ in range(2):
                            tq = th * 2 + t2_
                            pc = ps_next("M")
                            for tl in range(4):
                                ti = tq * 4 + tl
                                dg = dgs[tl % 2]
                                ts("dve", dg, ident_f, comb[:, ti, e_:e_ + 1], None, ALU.mult)
                                dgh_, dgl_ = dgh[tl % 2], dgl[tl % 2]
                                tcopy("dve", dgh_, dg)
                                tt("dve", dgl_, dg, dgh_, ALU.subtract)
                                mm(pc[:, tl * 128:(tl + 1) * 128], dr_ones1, dgh_, True, False)
                                mm(pc[:, tl * 128:(tl + 1) * 128], dr_ones1, dgl_, False, True)
                            tcopy("dve", cbt[t2_], pc)
                    for jj in range(2):
                        j = jp * 2 + jj
                        for t2_ in range(2):
                            tq = th * 2 + t2_
                            pA = ps_next("O")
                            for d in range(8):
                                mm(pA, wa[:, d, jj * 128:(jj + 1) * 128], hT[:, d, cs(tq)], d == 0, d == 7)
                            pB = ps_next("O")
                            for d in range(8):
                                mm(pB, wb[:, d, jj * 128:(jj + 1) * 128], hT[:, d, cs(tq)], d == 0, d == 7)
                            s_ = sg_[(j * 2 + t2_) % 2]
                            act(s_, pA, AF.Silu)
                            if moe:
                                t_ = tf[(j * 2 + t2_) % 2]
                                tt("dve", t_, s_, pB, ALU.mult)
                                tt("pool", uT[:, j, t2_ * 512:(t2_ + 1) * 512], t_, cbt[t2_], ALU.mult)
                            else:
                                tt("dve", uT[:, j, t2_ * 512:(t2_ + 1) * 512], s_, pB, ALU.mult)
                else:
                    (w2,) = bufs
                    fq = idx
                    for f2 in range(2):
                        fo = fq * 2 + f2
                        for t2_ in range(2):
                            tq = th * 2 + t2_
                            pO = ps_next("M")
                            for j in range(NJ):
                                mm(pO, w2[:, j, f2 * 128:(f2 + 1) * 128], uT[:, j, t2_ * 512:(t2_ + 1) * 512], j == 0, j == NJ - 1)
                            tt("dve", xT[:, fo, cs(tq)], xT[:, fo, cs(tq)], pO, ALU.add)

            loaded = {}
            loaded[0] = do_load(items[0])
            for i, it in enumerate(items):
                if i + 1 < len(items):
                    loaded[i + 1] = do_load(items[i + 1])
                do_compute(it, loaded.pop(i))
            top[0] = m0

        ones1 = alloc([128, 128], BF16)
        dma("sp", ones1, dr["c_ones1_f"])
        dr_ones1 = ones1
        PH0 = top[0]

        stages = ["mixer0", "ffn0", "mixer1", "ffn1", "full"]
        si = stages.index(stop_after) if stop_after in stages else 0
        try:
            mixer(0)
        except StopBuild:
            pass
        if si >= 1:
            ffn(0)
        if si >= 2:
            mixer(1)
        if si >= 3:
            ffn(1)
        outs = []
        if si >= 4:
            sq = [alloc([128, 512], F32) for _ in range(2)]
            sqh = [alloc([128, 512], BF16) for _ in range(2)]
            sql = [alloc([128, 512], BF16) for _ in range(2)]
            rstd = [alloc([128, 512], F32) for _ in range(2)]
            for tq in range(4):
                pm = ps_next("M")
                for f in range(8):
                    s_ = sq[f % 2]
                    act(s_, xT[:, f, cs(tq)], AF.Square)
                    tcopy("dve", sqh[f % 2], s_)
                    tt("dve", sql[f % 2], s_, sqh[f % 2], ALU.subtract)
                    mm(pm, ones_f, sqh[f % 2], f == 0, False)
                    mm(pm, ones_f, sql[f % 2], False, f == 7)
                r_ = rstd[tq % 2]
                ts("dve", r_, pm, EPS, None, ALU.add)
                act(r_, r_, AF.Sqrt)
                recip(r_, r_)
                for f in range(8):
                    stt("dve", xT[:, f, cs(tq)], xT[:, f, cs(tq)], gains[:, 4, f:f + 1], r_, ALU.mult, ALU.mult)
        for f in range(8):
            o = P.op("sp", lambda e, f=f: e.dma_start(out=outT[f * 128:(f + 1) * 128, :], in_=xT.ap[:, f, :]),
                     reads=[xT[:, f, :]], dma=True)
            outs.append(o)
        fin = P.op("sp", lambda e: e.nop())
        for o in outs:
            fin.deps.add(o.id)
        P.finalize(sems, dsems, block)
    nc._declared = set(dr.keys())
    return nc, P


_CACHE = {}


def make_in_maps(inputs):
    hc = host_consts()
    x = np.asarray(inputs["x"], np.float32)
    pos = np.asarray(inputs["positions"], np.int32)
    shared = dict(hc)
    for l in range(2):
        shared["w_in%d" % l] = np.ascontiguousarray(inputs["w_in"][l], dtype=np.float32)
        shared["g_mix%d" % l] = np.ascontiguousarray(inputs["norm_mix"][l], dtype=np.float32)
        shared["g_ffn%d" % l] = np.ascontiguousarray(inputs["norm_ffn"][l], dtype=np.float32)
        shared["posk%d" % l] = np.ascontiguousarray(inputs["cmp_pos_k"][l], dtype=np.float32)
        shared["posv%d" % l] = np.ascontiguousarray(inputs["cmp_pos_v"][l], dtype=np.float32)
        shared["kw1_%d" % l] = np.ascontiguousarray(inputs["cmp_k_w1"][l], dtype=np.float32)
        shared["kw2_%d" % l] = np.ascontiguousarray(inputs["cmp_k_w2"][l], dtype=np.float32)
        shared["vw1_%d" % l] = np.ascontiguousarray(inputs["cmp_v_w1"][l], dtype=np.float32)
        shared["vw2_%d" % l] = np.ascontiguousarray(inputs["cmp_v_w2"][l], dtype=np.float32)
        shared["pa%d" % l] = np.ascontiguousarray(inputs["w_branch_a"][l], dtype=np.float32)
        shared["pb%d" % l] = np.ascontiguousarray(inputs["w_branch_b"][l], dtype=np.float32)
        shared["wo%d" % l] = np.ascontiguousarray(inputs["w_out"][l], dtype=np.float32)
    shared["f_w1"] = np.ascontiguousarray(inputs["ffn_w1"], dtype=np.float32)
    shared["f_w3"] = np.ascontiguousarray(inputs["ffn_w3"], dtype=np.float32)
    shared["f_w2"] = np.ascontiguousarray(inputs["ffn_w2"], dtype=np.float32)
    shared["router"] = np.ascontiguousarray(inputs["router"][0], dtype=np.float32)
    shared["m_w1"] = np.ascontiguousarray(inputs["moe_w1"][0], dtype=np.float32)
    shared["m_w3"] = np.ascontiguousarray(inputs["moe_w3"][0], dtype=np.float32)
    shared["m_w2"] = np.ascontiguousarray(inputs["moe_w2"][0], dtype=np.float32)
    shared["g_fin"] = np.ascontiguousarray(inputs["final_norm"], dtype=np.float32)
    maps = []
    for b in range(8):
        m = dict(shared)
        m["xT"] = np.ascontiguousarray(x[b].T)
        m["pos"] = np.ascontiguousarray(pos[b:b + 1])
        maps.append(m)
    return maps


def kernel(stop_after="full", **inputs):
    if stop_after not in _CACHE:
        _CACHE[stop_after] = build_program(stop_after)[0]
    nc = _CACHE[stop_after]
    maps = make_in_maps(inputs)
    decl = nc._declared
    maps = [{k: v for k, v in m.items() if k in decl} for m in maps]
    res = run_bass_kernel_spmd(nc, maps, core_ids=list(range(8)))
    out = np.stack([np.ascontiguousarray(r["outT"].T) for r in res.results], axis=0)
    return out.astype(np.float32)
```

```python
import numpy as np
from contextlib import ExitStack
import concourse.bass as bass
import concourse.mybir as mybir
from concourse.bass_utils import run_bass_kernel_spmd

F32 = mybir.dt.float32
BF16 = mybir.dt.bfloat16
I32 = mybir.dt.int32
AF = mybir.ActivationFunctionType
ALU = mybir.AluOpType

GRAN = 256
N_DMA_SEM = 12
NEG = -30000.0
S = 2048
D = 1024
DFF = 2816
NJ = DFF // 128
IN_COLS = 5656
EPS = 1e-6


class Buf:
    def __init__(self, ap, lo, hi, space, shape=None, esz=2, exact=True):
        self.ap = ap
        self.lo = lo
        self.hi = hi
        self.space = space
        self.shape = shape
        self.esz = esz
        self.exact = exact and shape is not None

    def grans(self):
        if self.space == 'ps':
            return range((1 << 20) + self.lo // 2048, (1 << 20) + (self.hi - 1) // 2048 + 1)
        return range(self.lo // GRAN, (self.hi - 1) // GRAN + 1)

    def __getitem__(self, key):
        if not isinstance(key, tuple):
            key = (key,)
        ap = self.ap[key]
        if not self.exact:
            return Buf(ap, self.lo, self.hi, self.space, None, self.esz, False)
        key = key + (slice(None),) * (len(self.shape) - len(key))
        fs = self.shape[1:]
        strides = [1] * len(fs)
        for i in range(len(fs) - 2, -1, -1):
            strides[i] = strides[i + 1] * fs[i + 1]
        mn = mx = 0
        for i, k in enumerate(key[1:]):
            n = fs[i]
            if isinstance(k, int):
                a = b = k
            else:
                a, b, st = k.indices(n)
                b = a + ((b - a - 1) // st) * st
            mn += a * strides[i]
            mx += b * strides[i]
        return Buf(ap, self.lo + mn * self.esz, self.lo + (mx + 1) * self.esz, self.space, None, self.esz, False)


class Op:
    __slots__ = ("id", "eng", "emit", "deps", "is_dma", "val", "needed", "qeng")


class Prog:
    ENGS = ("pe", "act", "dve", "pool", "sp")

    def __init__(self):
        self.ops = []
        self.lastw = {}
        self.lastr = {}
        self.dma_rr = {"sp": 0, "pool": 0}
        self.dma_last = {"sp": [None] * N_DMA_SEM, "pool": [None] * N_DMA_SEM}

    def op(self, eng, emit, reads=(), writes=(), dma=False):
        o = Op()
        o.id = len(self.ops)
        o.emit = emit
        o.is_dma = dma
        o.qeng = eng
        o.needed = False
        o.val = None
        deps = set()
        if dma:
            k = self.dma_rr[eng]
            self.dma_rr[eng] = (k + 1) % N_DMA_SEM
            o.eng = ("d%d" if eng == "sp" else "g%d") % k
            if self.dma_last[eng][k] is not None:
                deps.add(self.dma_last[eng][k])
            self.dma_last[eng][k] = o.id
        else:
            o.eng = eng
        lw, lr = self.lastw, self.lastr
        for b in reads:
            for g in b.grans():
                w = lw.get(g)
                if w is not None:
                    deps.add(w)
        for b in writes:
            for g in b.grans():
                w = lw.get(g)
                if w is not None:
                    deps.add(w)
                rs = lr.get(g)
                if rs:
                    deps.update(rs.values())
        for b in reads:
            for g in b.grans():
                d = lr.get(g)
                if d is None:
                    lr[g] = {o.eng: o.id}
                else:
                    d[o.eng] = o.id
        for b in writes:
            for g in b.grans():
                lw[g] = o.id
                lr[g] = None
        deps.discard(o.id)
        o.deps = deps
        self.ops.append(o)
        return o

    def finalize(self, sems, dsems, block):
        ops = self.ops
        for o in ops:
            nd = set()
            for d in o.deps:
                do = ops[d]
                if do.eng == o.eng and o.eng == "pe":
                    continue
                nd.add(d)
                do.needed = True
            o.deps = nd
        cnt = {}
        for o in ops:
            if o.is_dma:
                o.needed = True
            if o.needed:
                step = 16 if o.is_dma else 1
                cnt[o.eng] = cnt.get(o.eng, 0) + step
                o.val = cnt[o.eng]
        self.final_counts = cnt
        queues = {e: [] for e in self.ENGS}
        for o in ops:
            queues[o.qeng].append(o)

        def semof(k):
            if k[0] == "d" and k[1:].isdigit():
                return dsems[int(k[1:])]
            if k[0] == "g" and k[1:].isdigit():
                return dsems[N_DMA_SEM + int(k[1:])]
            return sems[k]

        def run_queue(qname, engobj):
            waited = {}
            for o in queues[qname]:
                need = {}
                for d in o.deps:
                    do = ops[d]
                    if do.val > need.get(do.eng, 0):
                        need[do.eng] = do.val
                for ek, v in need.items():
                    if waited.get(ek, 0) >= v:
                        continue
                    engobj.wait_ge(semof(ek), v)
                    waited[ek] = v
                ins = o.emit(engobj)
                if o.needed:
                    ins.then_inc(semof(o.eng), 16 if o.is_dma else 1)

        @block.tensor
        def _(e):
            run_queue("pe", e)

        @block.scalar
        def _(e):
            run_queue("act", e)

        @block.vector
        def _(e):
            run_queue("dve", e)

        @block.gpsimd
        def _(e):
            run_queue("pool", e)

        @block.sync
        def _(e):
            run_queue("sp", e)


def host_consts():
    import ml_dtypes
    bf = ml_dtypes.bfloat16
    c = {}
    c["c_ident_bf"] = np.eye(128, dtype=np.float32).astype(bf)
    c["c_ident_f"] = np.eye(128, dtype=np.float32)
    c["c_ones_f"] = np.full((128, 128), 1.0 / D, dtype=np.float32).astype(bf)
    c["c_ones1_f"] = np.ones((128, 128), dtype=np.float32).astype(bf)
    rm = np.zeros((128, 128), np.float32)
    for m in range(128):
        r = m % 64
        if r < 8:
            rm[m + 8, m] = -1.0
        elif r < 16:
            rm[m - 8, m] = 1.0
    c["c_rmat"] = rm.astype(bf)
    inv = (500000.0 ** (-np.arange(0, 16, 2, dtype=np.float32) / 16.0)).astype(np.float32)
    invrow = np.zeros((128, 1), np.float32)
    for p in range(128):
        if p % 64 < 16:
            invrow[p, 0] = inv[p % 8]
    c["c_invrow"] = invrow
    k = np.arange(128)[:, None]

    def wtab(width, f):
        r = np.arange(width)[None, :]
        dl = r - 384 - k
        return np.where(f(dl), 0.0, NEG).astype(np.float32).astype(bf)

    c["c_wwin"] = wtab(1408, lambda d: (d >= 0) & (d <= 511))
    c["c_wcaus"] = wtab(896, lambda d: d >= 0)
    c["c_wg1"] = wtab(1024, lambda d: (d >= 0) & (d <= 128))
    c["c_wg2"] = wtab(1408, lambda d: (d >= 0) & (d <= 512) & (d % 4 == 0))
    c["c_wg3"] = wtab(1024, lambda d: (d >= 0) & (d % 16 == 0))
    t = np.arange(S)[None, :]
    cm = np.where((16 * k + 31 <= t) & (k < 127), 0.0, NEG).astype(np.float32)
    c["c_cmpmask"] = cm.astype(bf)
    E = np.zeros((32, S), np.float32)
    for j in range(32):
        E[j, 64 * j:64 * j + 64] = 1.0
    c["c_E"] = E.astype(bf)
    starts = np.arange(127) * 16
    bstart = np.arange(32) * 64
    ov = np.clip(np.minimum(starts[:, None] + 32, bstart[None, :] + 64) - np.maximum(starts[:, None], bstart[None, :]), 0, None)
    M = np.zeros((128, 33), np.float32)
    M[:127, :32] = ov / 32.0
    M[:, 32] = 1.0
    c["c_maug"] = M.astype(bf)
    fb = np.zeros((128, 16, 32), np.float32)
    for tile in range(16):
        for p in range(128):
            tt = tile * 128 + p
            cur = tt // 64
            for j in range(32):
                if j > cur:
                    v = -1e9
                elif j == 0:
                    v = 3e9
                elif j == cur:
                    v = 2e9
                elif j == cur - 1:
                    v = 1e9
                else:
                    v = 0.0
                fb[p, tile, j] = v
    c["c_forceb"] = fb
    sg = np.zeros((24, 12, 128), np.float32)
    for ch in range(4):
        for br in range(3):
            for m in range(128):
                sg[3 * (2 * ch + m // 64) + br, ch * 3 + br, m] = 1.0
    c["c_selgate"] = sg.astype(bf)
    return c


CONST_SHAPES = None


def build_program(stop_after="full"):
    nc = bass.Bass("TRN2", target_bir_lowering=False)
    P = Prog()
    hc = host_consts()

    def din(name, shape, dt=F32):
        return nc.dram_tensor(name, list(shape), dt, kind="ExternalInput").ap()

    dr = {}
    dr["xT"] = din("xT", [D, S])
    dr["pos"] = din("pos", [1, S], I32)
    for k_, v_ in hc.items():
        dr[k_] = din(k_, v_.shape, F32 if v_.dtype == np.float32 else BF16)
    for l in range(2):
        dr["w_in%d" % l] = din("w_in%d" % l, [D, IN_COLS])
        dr["g_mix%d" % l] = din("g_mix%d" % l, [D])
        dr["g_ffn%d" % l] = din("g_ffn%d" % l, [D])
        dr["posk%d" % l] = din("posk%d" % l, [32, 64])
        dr["posv%d" % l] = din("posv%d" % l, [32, 64])
        dr["kw1_%d" % l] = din("kw1_%d" % l, [2048, 128])
        dr["kw2_%d" % l] = din("kw2_%d" % l, [128, 64])
        dr["vw1_%d" % l] = din("vw1_%d" % l, [2048, 128])
        dr["vw2_%d" % l] = din("vw2_%d" % l, [128, 64])
        dr["pa%d" % l] = din("pa%d" % l, [512, D])
        dr["pb%d" % l] = din("pb%d" % l, [256, D])
        dr["wo%d" % l] = din("wo%d" % l, [D, D])
    _st = ["mixer0", "ffn0", "mixer1", "ffn1", "full"]
    _si = _st.index(stop_after) if stop_after in _st else 0
    if _si >= 1:
        dr["f_w1"] = din("f_w1", [1, D, DFF])
        dr["f_w3"] = din("f_w3", [1, D, DFF])
        dr["f_w2"] = din("f_w2", [1, DFF, D])
    if _si >= 3:
        dr["router"] = din("router", [D, 8])
        dr["m_w1"] = din("m_w1", [8, D, DFF])
        dr["m_w3"] = din("m_w3", [8, D, DFF])
        dr["m_w2"] = din("m_w2", [8, DFF, D])
    dr["g_fin"] = din("g_fin", [D])
    outT = nc.dram_tensor("outT", [D, S], F32, kind="ExternalOutput").ap()

    ARENA_BYTES = 212480
    with ExitStack() as st:
        arena = st.enter_context(nc.sbuf_tensor("arena", [128, ARENA_BYTES // 2], BF16))
        pst = [st.enter_context(nc.psum_tensor("ps%d" % i, [128, 512], F32)) for i in range(8)]
        sems = {e: st.enter_context(nc.semaphore("s_" + e)) for e in Prog.ENGS}
        dsems = [st.enter_context(nc.semaphore("dq%d" % i)) for i in range(2 * N_DMA_SEM)]
        block = st.enter_context(nc.Block())

        top = [0]

        def alloc(shape, dt):
            n = int(np.prod(shape[1:]))
            esz = 2 if dt == BF16 else 4
            nb = n * esz
            lo = (top[0] + 255) // 256 * 256
            assert lo + nb <= ARENA_BYTES, ("SBUF overflow", lo + nb)
            top[0] = lo + nb
            v = arena[:, lo // 2:(lo + nb) // 2]
            if dt != BF16:
                v = v.bitcast(dt)
            if len(shape) == 3:
                v = v.rearrange("p (a b) -> p a b", b=shape[2])
            elif len(shape) == 4:
                v = v.rearrange("p (a b c) -> p a b c", b=shape[2], c=shape[3])
            v = v[0:shape[0]]
            return Buf(v, lo, lo + nb, 'sb', tuple(shape), esz)

        PS = [Buf(pst[i][:], i * 2048, (i + 1) * 2048, 'ps', (128, 512), 4) for i in range(8)]
        rr = {"S": 0, "O": 0, "M": 0}

        def ps_next(kind):
            banks = {"S": (0, 1, 2), "O": (3, 4, 5), "M": (6, 7)}[kind]
            i = rr[kind]
            rr[kind] = (i + 1) % len(banks)
            return PS[banks[i]]

        def bufs_of(*xs):
            return [x for x in xs if isinstance(x, Buf)]

        def sc(x):
            return x.ap if isinstance(x, Buf) else x

        def mm(out, lhsT, rhs, start, stop):
            P.op("pe", lambda e: e.matmul(out.ap, lhsT=lhsT.ap, rhs=rhs.ap, start=start, stop=stop,
                                          skip_group_check=True), reads=[lhsT, rhs], writes=[out])

        def act(out, in_, func, scale=1.0, bias=None):
            if bias is None:
                P.op("act", lambda e: e.activation(out=out.ap, in_=in_.ap, func=func, scale=scale),
                     reads=[in_], writes=[out])
            else:
                P.op("act", lambda e: e.activation(out=out.ap, in_=in_.ap, func=func, scale=scale, bias=bias.ap),
                     reads=[in_, bias], writes=[out])

        def tcopy(eng, out, in_):
            P.op(eng, lambda e: e.tensor_copy(out=out.ap, in_=in_.ap), reads=[in_], writes=[out])

        def tt(eng, out, a, b, op):
            P.op(eng, lambda e: e.tensor_tensor(out=out.ap, in0=a.ap, in1=b.ap, op=op), reads=[a, b], writes=[out])

        def ts(eng, out, a, s1, s2, op0, op1=None):
            if op1 is None:
                P.op(eng, lambda e: e.tensor_scalar(out=out.ap, in0=a.ap, scalar1=sc(s1), scalar2=None, op0=op0),
                     reads=[a] + bufs_of(s1), writes=[out])
            else:
                P.op(eng, lambda e: e.tensor_scalar(out=out.ap, in0=a.ap, scalar1=sc(s1), scalar2=sc(s2), op0=op0, op1=op1),
                     reads=[a] + bufs_of(s1, s2), writes=[out])

        def stt(eng, out, a, s, b, op0, op1):
            P.op(eng, lambda e: e.scalar_tensor_tensor(out=out.ap, in0=a.ap, scalar=sc(s), in1=b.ap, op0=op0, op1=op1),
                 reads=[a, b] + bufs_of(s), writes=[out])

        def recip(out, in_):
            P.op("dve", lambda e: e.reciprocal(out=out.ap, in_=in_.ap), reads=[in_], writes=[out])

        def memset(eng, b, val):
            P.op(eng, lambda e: e.memset(b.ap, val), writes=[b])

        def dma(q, out_buf, in_ap, **kw):
            return P.op(q, lambda e: e.dma_start(out=out_buf.ap, in_=in_ap, **kw), writes=[out_buf], dma=True)

        def wload(out_buf, in_ap, **kw):
            return dma("pool", out_buf, in_ap, **kw)

        xT = alloc([128, 8, S], F32)
        hT = alloc([128, 8, S], BF16)
        ident_bf = alloc([128, 128], BF16)
        ident_f = alloc([128, 128], F32)
        ones_f = alloc([128, 128], BF16)
        rmat = alloc([128, 128], BF16)
        invrow = alloc([128, 1], F32)
        gains = alloc([128, 5, 8], F32)
        posi = None
        dma("sp", ident_bf, dr["c_ident_bf"])
        dma("sp", ident_f, dr["c_ident_f"])
        dma("sp", ones_f, dr["c_ones_f"])
        dma("sp", rmat, dr["c_rmat"])
        dma("sp", invrow, dr["c_invrow"])
        for i, nm in enumerate(["g_mix0", "g_ffn0", "g_mix1", "g_ffn1", "g_fin"]):
            dma("sp", gains[:, i, :], dr[nm].rearrange("(c p) -> p c", p=128), allow_slow_non_contiguous=True)
        for f in range(8):
            dma("sp", xT[:, f, :], dr["xT"][f * 128:(f + 1) * 128, :])
        PH0 = top[0]

        def cs(tq):
            return slice(tq * 512, (tq + 1) * 512)

        def rmsnorm(gain_idx, rstd_keep=None):
            m0 = top[0]
            sq = [alloc([128, 512], F32) for _ in range(2)]
            sqh = [alloc([128, 512], BF16) for _ in range(2)]
            sql = [alloc([128, 512], BF16) for _ in range(2)]
            rs_tmp = [alloc([128, 512], F32) for _ in range(2)]
            for tq in range(4):
                pm = ps_next("M")
                for f in range(8):
                    s_ = sq[f % 2]
                    act(s_, xT[:, f, cs(tq)], AF.Square)
                    tcopy("dve", sqh[f % 2], s_)
                    tt("dve", sql[f % 2], s_, sqh[f % 2], ALU.subtract)
                    mm(pm, ones_f, sqh[f % 2], f == 0, False)
                    mm(pm, ones_f, sql[f % 2], False, f == 7)
                rstd = rstd_keep[tq] if rstd_keep is not None else rs_tmp[tq % 2]
                ts("dve", rstd, pm, EPS, None, ALU.add)
                act(rstd, rstd, AF.Sqrt)
                recip(rstd, rstd)
                for f in range(8):
                    stt("dve", hT[:, f, cs(tq)], xT[:, f, cs(tq)], gains[:, gain_idx, f:f + 1], rstd, ALU.mult, ALU.mult)
            top[0] = m0

        def proj_fm(wt, wsl, tq, ps):
            for d in range(8):
                mm(ps, wt[:, d, wsl], hT[:, d, cs(tq)], d == 0, d == 7)

        def rope_evac(ps, dst, cosT, sinT, tq, tmps):
            import os as _os
            lvl = int(_os.environ.get("DBG_ROPE", "5"))
            raw, t1, t2 = tmps
            if lvl >= 1:
                tcopy("dve", raw, ps)
            if lvl >= 2:
                pp = ps_next("M")
                mm(pp, rmat, raw, True, True)
            if lvl >= 3:
                tt("dve", t1, ps, cosT[:, cs(tq)], ALU.mult)
            if lvl >= 4:
                tt("dve", t2, pp, sinT[:, cs(tq)], ALU.mult)
            if lvl >= 5:
                tt("dve", dst, t1, t2, ALU.add)
            else:
                tcopy("dve", dst, ps)

        def attn_run(calls, ptiles, pidx):
            flat = []
            for ci, (steps, fin) in enumerate(calls):
                fi = [i for i, s_ in enumerate(steps) if s_["j0"] == 0 and s_["j1"] == 512][0]
                steps = [steps[fi]] + steps[:fi] + steps[fi + 1:]
                for si, stp in enumerate(steps):
                    flat.append((ci, si, len(steps), stp, fin))

            def emit_qk(stp):
                j0, j1 = stp["j0"], stp["j1"]
                pS = ps_next("S")
                ml = stp["masks"]
                mm(pS[:, j0:j1], stp["k"], stp["q"], True, len(ml) == 0)
                for mi, mk_ in enumerate(ml):
                    a_, b_ = mk_[0], mk_[1]
                    m0_, m1_ = (mk_[2], mk_[3]) if len(mk_) == 4 else (j0, j1)
                    mm(pS[:, m0_:m1_], a_, b_, False, mi == len(ml) - 1)
                return pS

            LOOK = 2
            pos = {}
            pend = []
            nq = 0
            while nq < min(LOOK, len(flat)):
                pend.append(emit_qk(flat[nq][3]))
                nq += 1
            for gi, (ci, si, n, stp, fin) in enumerate(flat):
                pS = pend.pop(0)
                if nq < len(flat):
                    pend.append(emit_qk(flat[nq][3]))
                    nq += 1
                if si == 0:
                    pos[ci] = ps_next("O")
                po = pos[ci]
                j0, j1 = stp["j0"], stp["j1"]
                pt = ptiles[pidx[0] % len(ptiles)]
                pidx[0] += 1
                act(pt[:, j0:j1], pS[:, j0:j1], AF.Exp, scale=0.125)
                mm(po[:, j0:j1], stp["v"], pt[:, j0:j1], si == 0, si == n - 1)
                if si == n - 1:
                    fin(po)

        class StopBuild(Exception):
            pass

        def chk(name):
            if stop_after == name:
                raise StopBuild()

        def mixer(l):
            m_layer = top[0]
            w_in = dr["w_in%d" % l].rearrange("(c p) n -> p c n", p=128)
            cosT = alloc([128, S], F32)
            sinT = alloc([128, S], F32)
            m1 = top[0]
            posi_ = alloc([128, S], I32)
            ang = alloc([128, S], F32)
            wv_ = alloc([128, S], F32)
            mk_ = alloc([128, S], F32)
            dma("sp", posi_, dr["pos"].partition_broadcast(128))
            tcopy("dve", ang, posi_)
            ts("dve", ang, ang, invrow, float(1.0 / (2 * np.pi)), ALU.mult, ALU.mult)
            for (dst, shift) in ((sinT, 0.0), (cosT, 0.25)):
                if shift != 0.0:
                    ts("dve", ang, ang, shift, None, ALU.add)
                tcopy("dve", posi_, ang)
                tcopy("dve", wv_, posi_)
                tt("dve", wv_, ang, wv_, ALU.subtract)
                ts("dve", mk_, wv_, 0.5, None, ALU.is_gt)
                tt("dve", wv_, wv_, mk_, ALU.subtract)
                ts("dve", mk_, wv_, -0.5, None, ALU.is_lt)
                tt("dve", wv_, wv_, mk_, ALU.add)
                act(dst, wv_, AF.Sin, scale=6.28318)
            top[0] = m1

            rmsnorm(2 * l)
            if stop_after == "m_pre":
                top[0] = m_layer
                return

            oa = [alloc([128, S], BF16) for _ in range(4)]
            ob = [alloc([128, S], BF16) for _ in range(2)]
            m_attn = top[0]

            wwin = alloc([128, 1408], BF16)
            wcaus = alloc([128, 896], BF16)
            cmpmask = alloc([128, S], BF16)
            Emat = alloc([32, S], BF16)
            maug = alloc([128, 33], BF16)
            forceb = alloc([128, 16, 32], F32)
            selgate = alloc([24, 12, 128], BF16)
            for b_, nm in ((wwin, "c_wwin"), (wcaus, "c_wcaus"), (cmpmask, "c_cmpmask"), (Emat, "c_E"),
                           (maug, "c_maug"), (forceb, "c_forceb"), (selgate, "c_selgate")):
                dma("sp", b_, dr[nm])
            GT = alloc([24, S], BF16)
            ptiles = [alloc([128, 512], BF16) for _ in range(3)]
            raw = alloc([128, 512], BF16)
            t1 = alloc([128, 512], F32)
            t2 = alloc([128, 512], F32)
            rtmps = (raw, t1, t2)
            rd = t1
            on = t2
            wst = [alloc([128, 8, 128], BF16) for _ in range(2)]
            wsi = [0]

            def next_w():
                w = wst[wsi[0] % len(wst)]
                wsi[0] += 1
                return w

            wg = next_w()
            wload(wg, w_in[:, :, 1280:1408])
            for tq in range(4):
                pg = ps_next("M")
                for d in range(8):
                    mm(pg[0:24, :], wg[:, d, 0:24], hT[:, d, cs(tq)], d == 0, d == 7)
                act(GT[:, cs(tq)], pg[0:24, :], AF.Sigmoid)
            chk("n0")
            kcT2 = [alloc([128, 128], BF16) for _ in range(2)]
            vaug_cmp = [alloc([128, 128], BF16) for _ in range(2)]
            m_grp = top[0]
            kcmpT = alloc([128, S], BF16)
            vcmpT = alloc([128, S], BF16)
            w1k = alloc([128, 32, 128], BF16)
            w1v = alloc([128, 32, 128], BF16)
            w2k = alloc([128, 128], BF16)
            w2v = alloc([128, 64], BF16)
            pkT = alloc([128, 32], BF16)
            pvT = alloc([128, 32], BF16)
            biask = alloc([128, 1], F32)
            biasv = alloc([128, 1], F32)
            hsk = alloc([128, 128], BF16)
            hsv = alloc([128, 128], BF16)
            wk_ = next_w()
            wload(wk_, w_in[:, :, 512:640])
            for tq in range(4):
                pk = ps_next("M")
                proj_fm(wk_, slice(0, 128), tq, pk)
                rope_evac(pk, kcmpT[:, cs(tq)], cosT, sinT, tq, rtmps)
            chk("p1")
            wv = next_w()
            wload(wv, w_in[:, :, 640:768])
            for tq in range(4):
                pv = ps_next("M")
                proj_fm(wv, slice(0, 128), tq, pv)
                tcopy("dve", vcmpT[:, cs(tq)], pv)
            chk("p2")
            for half in range(2):
                sl = slice(half * 64, half * 64 + 64)
                for l4 in range(4):
                    wload(w1k[sl, l4 * 8:(l4 + 1) * 8, :], dr["kw1_%d" % l].rearrange("(l d) h -> d l h", d=64)[:, l4 * 8:(l4 + 1) * 8, :])
                    wload(w1v[sl, l4 * 8:(l4 + 1) * 8, :], dr["vw1_%d" % l].rearrange("(l d) h -> d l h", d=64)[:, l4 * 8:(l4 + 1) * 8, :])
            chk("p3")
            w2st = alloc([128, 128], F32)
            dma("sp", w2st[:, 0:64], dr["kw2_%d" % l])
            dma("sp", w2st[:, 64:128], dr["vw2_%d" % l])
            tcopy("dve", w2k[:, 0:64], w2st[:, 0:64])
            tcopy("dve", w2k[:, 64:128], w2st[:, 0:64])
            tcopy("dve", w2v, w2st[:, 64:128])
            chk("p4")
            posn = alloc([32, 128], F32)
            dma("sp", posn[:, 0:64], dr["posk%d" % l])
            dma("sp", posn[:, 64:128], dr["posv%d" % l])
            posnb = alloc([32, 128], BF16)
            tcopy("dve", posnb, posn)
            for (pxT, c0_) in ((pkT, 0), (pvT, 64)):
                ptp = ps_next("M")
                mm(ptp[0:64, 0:32], posnb[:, c0_:c0_ + 64], ident_bf[0:32, 0:32], True, True)
                tcopy("dve", pxT[0:64, :], ptp[0:64, 0:32])
            chk("n1")
            for (w1x, pxT, bx) in ((w1k, pkT, biask), (w1v, pvT, biasv)):
                pb_ = ps_next("M")
                for li in range(32):
                    mm(pb_[:, 0:1], w1x[0:64, li, :], pxT[0:64, li:li + 1], li == 0, li == 31)
                tcopy("dve", bx, pb_[:, 0:1])
            for g in range(2):
                gs = slice(g * 64, g * 64 + 64)
                memset("pool", kcT2[g], 0.0)
                memset("pool", vaug_cmp[g], 0.0)
                memset("pool", vaug_cmp[g][:, 64:128], 1.0)
                ph = ps_next("M")
                for li in range(32):
                    mm(ph[:, 0:127], w1k[gs, li, :], kcmpT[gs, li:li + 16 * 126 + 1:16], li == 0, li == 31)
                act(hsk[:, 0:127], ph[:, 0:127], AF.Silu, bias=biask)
                pk2 = ps_next("M")
                mm(pk2[:, 0:127], w2k, hsk[:, 0:127], True, True)
                tcopy("dve", kcT2[g][:, 0:127], pk2[:, 0:127])
                ph = ps_next("M")
                for li in range(32):
                    mm(ph[:, 0:127], w1v[gs, li, :], vcmpT[gs, li:li + 16 * 126 + 1:16], li == 0, li == 31)
                act(hsv[:, 0:127], ph[:, 0:127], AF.Silu, bias=biasv)
                pv2 = ps_next("M")
                mm(pv2[0:127, 0:64], hsv[:, 0:127], w2v, True, True)
                tcopy("dve", vaug_cmp[g][0:127, 0:64], pv2[0:127, 0:64])

            chk("n2")
            for g in range(2):
                top[0] = m_grp
                kT = [None] + [alloc([128, S], BF16) for _ in range(2)]
                qT1 = alloc([128, S], BF16)
                vaug_sel = alloc([128, 16, 128], BF16)
                vaug_win = alloc([128, 16, 128], BF16)
                imp = alloc([128, 16, 32], F32)
                negsel = alloc([128, 32], BF16)
                negselT = alloc([32, S], BF16)
                m8 = alloc([128, 16], F32)
                mrep = alloc([128, 32], F32)
                rdn = alloc([128, 4], F32)
                wdup = alloc([128, 8, 128], BF16)
                for bi in (1, 2):
                    col = 512 + bi * 256 + g * 64
                    wl_ = next_w()
                    col0 = 512 + bi * 256
                    wload(wl_, w_in[:, :, col0:col0 + 128])
                    w = wdup
                    tcopy("dve", w[:, :, 0:64], wl_[:, :, g * 64:g * 64 + 64])
                    tcopy("dve", w[:, :, 64:128], wl_[:, :, g * 64:g * 64 + 64])
                    for tq in range(4):
                        pk = ps_next("M")
                        proj_fm(w, slice(0, 128), tq, pk)
                        rope_evac(pk, kT[bi][:, cs(tq)], cosT, sinT, tq, rtmps)
                w = wdup
                wl_ = next_w()
                wload(wl_, w_in[:, :, 896:1024])
                tcopy("dve", w[:, :, 0:64], wl_[:, :, g * 64:g * 64 + 64])
                wl_ = next_w()
                wload(wl_, w_in[:, :, 1152:1280])
                tcopy("dve", w[:, :, 64:128], wl_[:, :, g * 64:g * 64 + 64])
                memset("pool", vaug_sel[:, :, 64:128], 1.0)
                memset("pool", vaug_win[:, :, 64:128], 1.0)
                for t4 in range(4):
                    pv = ps_next("M")
                    for tl in range(4):
                        ti = t4 * 4 + tl
                        for d in range(8):
                            mm(pv[:, tl * 128:(tl + 1) * 128], hT[:, d, ti * 128:(ti + 1) * 128], w[:, d, :], d == 0, d == 7)
                    pv3 = Buf(pv.ap.rearrange("p (a b) -> p a b", b=128), pv.lo, pv.hi, 'ps')
                    tcopy("dve", vaug_sel[:, t4 * 4:(t4 + 1) * 4, 0:64], pv3[:, :, 0:64])
                    tcopy("dve", vaug_win[:, t4 * 4:(t4 + 1) * 4, 0:64], pv3[:, :, 64:128])

                def proj_q(cc):
                    c_ = 2 * g + cc
                    w_ = next_w()
                    wload(w_, w_in[:, :, c_ * 128:(c_ + 1) * 128])
                    for tq_ in range(4):
                        pq = ps_next("M")
                        proj_fm(w_, slice(0, 128), tq_, pq)
                        rope_evac(pq, qT1[:, cs(tq_)], cosT, sinT, tq_, rtmps)
                chk("n3")
                tcopy("pool", imp, forceb)

                def finish(po, c, hh, br, tq, first):
                    hs_ = slice(hh * 64, hh * 64 + 64)
                    ts("dve", rd[64:128, :], po[64:128, :], 1e-30, None, ALU.max)
                    recip(rd[64:128, :], rd[64:128, :])
                    tt("dve", on[hs_, :], po[0:64, :], rd[64:128, :], ALU.mult)
                    pg_ = ps_next("M")
                    mm(pg_, selgate[:, c * 3 + br, :], GT[:, cs(tq)], True, True)
                    if first:
                        tt("dve", oa[c][hs_, cs(tq)], on[hs_, :], pg_[hs_, :], ALU.mult)
                    else:
                        tt("dve", on[hs_, :], on[hs_, :], pg_[hs_, :], ALU.mult)
                        tt("dve", oa[c][hs_, cs(tq)], oa[c][hs_, cs(tq)], on[hs_, :], ALU.add)

                pidx = [0]
                for i in range(4):
                    cc, hh = i // 2, i % 2
                    c = 2 * g + cc
                    hs_ = slice(hh * 64, hh * 64 + 64)
                    if hh == 0:
                        proj_q(cc)
                    for tq in range(4):
                        pS = ps_next("S")
                        mm(pS, kcT2[g][hs_, :], qT1[hs_, cs(tq)], True, False)
                        mm(pS, ident_bf, cmpmask[:, cs(tq)], False, True)
                        pt = ptiles[pidx[0] % 3]
                        pidx[0] += 1
                        act(pt, pS, AF.Exp, scale=0.125)
                        po = ps_next("O")
                        mm(po, vaug_cmp[g], pt, True, True)
                        pI = ps_next("M")
                        for ql in range(4):
                            mm(pI[:, ql * 33:(ql + 1) * 33], pt[:, ql * 128:(ql + 1) * 128], maug, True, True)
                        for ql in range(4):
                            qt_ = tq * 4 + ql
                            ts("dve", rdn[:, ql:ql + 1], pI[:, ql * 33 + 32:ql * 33 + 33], 1e-30, None, ALU.max)
                            recip(rdn[:, ql:ql + 1], rdn[:, ql:ql + 1])
                            stt("dve", imp[:, qt_, :], pI[:, ql * 33:ql * 33 + 32], rdn[:, ql:ql + 1], imp[:, qt_, :], ALU.mult, ALU.add)
                        finish(po, c, hh, 0, tq, True)
                chk("n4")
                for tq in range(2, 4):
                    pT = ps_next("M")
                    for ql in range(4):
                        qt_ = tq * 4 + ql
                        P.op("dve", lambda e, qt_=qt_: e.max(out=m8.ap[:, 0:8], in_=imp.ap[:, qt_, :]), reads=[imp[:, qt_, :]], writes=[m8[:, 0:8]])
                        P.op("dve", lambda e, qt_=qt_: e.match_replace(out=mrep.ap, in_to_replace=m8.ap[:, 0:8], in_values=imp.ap[:, qt_, :], imm_value=-3e9),
                             reads=[imp[:, qt_, :], m8[:, 0:8]], writes=[mrep])
                        P.op("dve", lambda e: e.max(out=m8.ap[:, 8:16], in_=mrep.ap), reads=[mrep], writes=[m8[:, 8:16]])
                        ts("dve", negsel, imp[:, qt_, :], m8[:, 15:16], None, ALU.is_lt)
                        ts("dve", negsel, negsel, NEG, None, ALU.mult)
                        mm(pT[0:32, ql * 128:(ql + 1) * 128], negsel, ident_bf, True, True)
                    tcopy("dve", negselT[:, cs(tq)], pT[0:32, :])
                chk("n5")
                for i in range(4):
                    cc, hh = i // 2, i % 2
                    c = 2 * g + cc
                    hs_ = slice(hh * 64, hh * 64 + 64)
                    if hh == 0:
                        proj_q(cc)
                        calls = []
                    for tq in range(4):
                        steps = []
                        kts = list(range(4 * tq + 4))
                        first = 4 * tq
                        kts = [first] + [k_ for k_ in kts if k_ != first]
                        for kt in kts:
                            dl = 512 * tq - 128 * kt
                            j0 = max(0, -dl)
                            j1 = 512
                            q0 = tq * 512
                            masks = [] if tq < 2 else [(Emat[:, kt * 128:(kt + 1) * 128], negselT[:, q0 + j0:q0 + j1])]
                            if kt >= 4 * tq:
                                masks.append((ident_bf, wcaus[:, dl + 384 + j0:dl + 384 + j0 + 128], j0, j0 + 128))
                            steps.append(dict(j0=j0, j1=j1, k=kT[1][hs_, kt * 128:(kt + 1) * 128],
                                              q=qT1[hs_, q0 + j0:q0 + j1], v=vaug_sel[:, kt, :], masks=masks))
                        calls.append((steps, lambda po, c=c, hh=hh, tq=tq: finish(po, c, hh, 1, tq, False)))
                        steps = []
                        kts = list(range(max(0, 4 * tq - 4), 4 * tq + 4))
                        kts = [first] + [k_ for k_ in kts if k_ != first]
                        for kt in kts:
                            dl = 512 * tq - 128 * kt
                            j0 = max(0, -dl)
                            j1 = min(512, 639 - dl)
                            q0 = tq * 512
                            if dl > 0:
                                m0_, m1_ = max(j0, 512 - dl), j1
                            else:
                                m0_, m1_ = j0, min(j1, j0 + 128)
                            masks = [(ident_bf, wwin[:, dl + 384 + m0_:dl + 384 + m1_], m0_, m1_)]
                            steps.append(dict(j0=j0, j1=j1, k=kT[2][hs_, kt * 128:(kt + 1) * 128],
                                              q=qT1[hs_, q0 + j0:q0 + j1], v=vaug_win[:, kt, :], masks=masks))
                        calls.append((steps, lambda po, c=c, hh=hh, tq=tq: finish(po, c, hh, 2, tq, False)))
                    if hh == 1:
                        attn_run(calls, ptiles, pidx)

            if stop_after == "m_nsa":
                top[0] = m_layer
                return
            top[0] = m_attn
            wg1 = alloc([128, 1024], BF16)
            wg2 = alloc([128, 1408], BF16)
            wg3 = alloc([128, 1024], BF16)
            for b_, nm in ((wg1, "c_wg1"), (wg2, "c_wg2"), (wg3, "c_wg3")):
                dma("sp", b_, dr[nm])
            ptiles = [alloc([128, 512], BF16) for _ in range(3)]
            raw = alloc([128, 512], BF16)
            t1 = alloc([128, 512], F32)
            t2 = alloc([128, 512], F32)
            rtmps = (raw, t1, t2)
            rd = t1
            wst = [alloc([128, 8, 128], BF16) for _ in range(2)]
            wsi = [0]
            qb = [alloc([128, S], BF16) for _ in range(3)]
            kb = [alloc([128, S], BF16) for _ in range(3)]
            vb = [[alloc([128, 16, 128], BF16) for _ in range(2)] for _ in range(3)]
            pidx = [0]
            WTAB = (wg1, wg2, wg3)
            WIN = (128, 512, 2048)
            for hp in range(2):
                for g in range(3):
                    for kind, dstl in ((0, qb), (1, kb)):
                        col = 1304 + kind * 768 + g * 256 + hp * 128
                        w = next_w()
                        wload(w, w_in[:, :, col:col + 128])
                        for tq in range(4):
                            pq = ps_next("M")
                            proj_fm(w, slice(0, 128), tq, pq)
                            rope_evac(pq, dstl[g][:, cs(tq)], cosT, sinT, tq, rtmps)
                    col = 1304 + 2 * 768 + g * 256 + hp * 128
                    w = next_w()
                    wload(w, w_in[:, :, col:col + 128])
                    for hh in range(2):
                        memset("pool", vb[g][hh][:, :, 64:128], 1.0)
                    for t4 in range(4):
                        pv = ps_next("M")
                        for tl in range(4):
                            ti = t4 * 4 + tl
                            for d in range(8):
                                mm(pv[:, tl * 128:(tl + 1) * 128], hT[:, d, ti * 128:(ti + 1) * 128], w[:, d, :], d == 0, d == 7)
                        pv3 = Buf(pv.ap.rearrange("p (a b) -> p a b", b=128), pv.lo, pv.hi, 'ps')
                        for hh in range(2):
                            tcopy("dve", vb[g][hh][:, t4 * 4:(t4 + 1) * 4, 0:64], pv3[:, :, hh * 64:hh * 64 + 64])
                calls = []

                def fin_dil(po, hp, hs_, tq):
                    recip(rd[64:128, :], po[64:128, :])
                    tt("dve", ob[hp][hs_, cs(tq)], po[0:64, :], rd[64:128, :], ALU.mult)

                for hh in range(2):
                    hs_ = slice(hh * 64, hh * 64 + 64)
                    for tq in range(4):
                        steps = []
                        q0 = tq * 512
                        for g in range(3):
                            wn = WIN[g]
                            lo_kt = max(0, (512 * tq - wn) // 128)
                            kts = list(range(lo_kt, 4 * tq + 4))
                            if g == 0:
                                kts = [4 * tq] + [k_ for k_ in kts if k_ != 4 * tq]
                            for kt in kts:
                                dl = 512 * tq - 128 * kt
                                j0 = max(0, -dl)
                                j1 = min(512, wn + 128 - dl)
                                if j1 <= j0:
                                    continue
                                dlm = min(dl, 128) if g == 2 else dl
                                masks = [(ident_bf, WTAB[g][:, dlm + 384 + j0:dlm + 384 + j1])]
                                steps.append(dict(j0=j0, j1=j1, k=kb[g][hs_, kt * 128:(kt + 1) * 128],
                                                  q=qb[g][hs_, q0 + j0:q0 + j1], v=vb[g][hh][:, kt, :], masks=masks))
                        calls.append((steps, lambda po, hp=hp, hs_=hs_, tq=tq: fin_dil(po, hp, hs_, tq)))
                attn_run(calls, ptiles, pidx)

            if stop_after == "m_dil":
                top[0] = m_layer
                return
            top[0] = m_attn
            pa = alloc([128, 4, D], BF16)
            pb = alloc([128, 2, D], BF16)
            yT = alloc([128, 8, S], BF16)
            g0 = alloc([128, 512], F32)
            g1 = alloc([128, 512], F32)
            u1 = alloc([128, 512], F32)
            u2 = alloc([128, 512], F32)
            wst = [alloc([128, 8, 128], BF16) for _ in range(4)]
            wload(pa, dr["pa%d" % l].rearrange("(c p) n -> p c n", p=128))
            wload(pb, dr["pb%d" % l].rearrange("(c p) n -> p c n", p=128))
            for f in range(8):
                fs_ = slice(f * 128, (f + 1) * 128)
                wm0 = next_w()
                wm1 = next_w()
                wload(wm0, w_in[:, :, 3608 + f * 128:3608 + (f + 1) * 128])
                wload(wm1, w_in[:, :, 4632 + f * 128:4632 + (f + 1) * 128])
                for tq in range(4):
                    pA = ps_next("O")
                    for c in range(4):
                        mm(pA, pa[:, c, fs_], oa[c][:, cs(tq)], c == 0, c == 3)
                    pB = ps_next("O")
                    for c in range(2):
                        mm(pB, pb[:, c, fs_], ob[c][:, cs(tq)], c == 0, c == 1)
                    pC = ps_next("S")
                    proj_fm(wm0, slice(0, 128), tq, pC)
                    pD = ps_next("S")
                    proj_fm(wm1, slice(0, 128), tq, pD)
                    act(g0, pC, AF.Sigmoid)
                    act(g1, pD, AF.Sigmoid)
                    tt("dve", u1, g0, pA, ALU.mult)
                    tt("dve", u2, g1, pB, ALU.mult)
                    tt("pool", yT[:, f, cs(tq)], u1, u2, ALU.add)
            top[0] = m_attn
            pa_ = alloc([128, 4, D], BF16)
            pb_ = alloc([128, 2, D], BF16)
            yT = alloc([128, 8, S], BF16)
            wo = alloc([128, 8, D], BF16)
            wload(wo, dr["wo%d" % l].rearrange("(c p) n -> p c n", p=128))
            for fo in range(8):
                for tq in range(4):
                    pO = ps_next("O")
                    for f in range(8):
                        mm(pO, wo[:, f, fo * 128:(fo + 1) * 128], yT[:, f, cs(tq)], f == 0, f == 7)
                    tt("dve", xT[:, fo, cs(tq)], xT[:, fo, cs(tq)], pO, ALU.add)
            top[0] = m_layer

        def ffn(l):
            m0 = top[0]
            moe = (l == 1)
            rstdF = [alloc([128, 512], F32) for _ in range(4)]
            rmsnorm(2 * l + 1, rstd_keep=rstdF)
            uT = alloc([128, NJ, 1024], BF16)
            w13 = [alloc([128, 8, 256], BF16) for _ in range(4)]
            w2b = [alloc([128, NJ, 256], BF16) for _ in range(2)]
            sg_ = [alloc([128, 512], BF16) for _ in range(2)]
            tf = [alloc([128, 512], F32) for _ in range(2)]
            cbt = [alloc([128, 512], F32) for _ in range(2)]
            wi = [0, 0]
            if moe:
                comb = alloc([128, 16, 8], F32)
                rg = alloc([128, 8, 8], F32)
                lg = alloc([128, 16], F32)
                lgs = alloc([128, 8], F32)
                m8 = alloc([128, 8], F32)
                sm = alloc([128, 8], F32)
                c0 = alloc([128, 8], F32)
                c1 = alloc([128, 8], F32)
                dgs = [alloc([128, 128], F32) for _ in range(2)]
                dgh = [alloc([128, 128], BF16) for _ in range(2)]
                dgl = [alloc([128, 128], BF16) for _ in range(2)]
                dma("sp", rg, dr["router"].rearrange("(c p) e -> p c e", p=128))
                for f in range(8):
                    ts("dve", rg[:, f, :], rg[:, f, :], gains[:, 2 * l + 1, f:f + 1], None, ALU.mult)
                rgh = alloc([128, 8, 8], BF16)
                rgl = alloc([128, 8, 8], BF16)
                tcopy("dve", rgh, rg)
                tt("dve", rgl, rg, rgh, ALU.subtract)
                xh = [alloc([128, 128], BF16) for _ in range(2)]
                xl = [alloc([128, 128], BF16) for _ in range(2)]
                for ti in range(16):
                    tq, tl = ti // 4, ti % 4
                    pr = ps_next("M")
                    for f in range(8):
                        xh_, xl_ = xh[f % 2], xl[f % 2]
                        tcopy("dve", xh_, xT[:, f, ti * 128:(ti + 1) * 128])
                        tt("dve", xl_, xT[:, f, ti * 128:(ti + 1) * 128], xh_, ALU.subtract)
                        mm(pr[:, 0:8], xh_, rgh[:, f, :], f == 0, False)
                        mm(pr[:, 0:8], xh_, rgl[:, f, :], False, False)
                        mm(pr[:, 0:8], xl_, rgh[:, f, :], False, f == 7)
                    xh_, xl_ = xh[0], xl[0]
                    tcopy("dve", xh_, rstdF[tq][:, tl * 128:(tl + 1) * 128])
                    tt("dve", xl_, rstdF[tq][:, tl * 128:(tl + 1) * 128], xh_, ALU.subtract)
                    mm(pr[:, 8:9], xh_, ident_bf[:, 0:1], False, False)
                    mm(pr[:, 8:9], xl_, ident_bf[:, 0:1], False, True)
                    tcopy("dve", lg[:, 0:9], pr[:, 0:9])
                    ts("dve", lgs, lg[:, 0:8], lg[:, 8:9], None, ALU.mult)
                    P.op("dve", lambda e: e.max(out=m8.ap, in_=lgs.ap), reads=[lgs], writes=[m8])
                    tt("dve", sm[:, 0:1], m8[:, 1:2], m8[:, 0:1], ALU.subtract)
                    act(sm[:, 1:2], sm[:, 0:1], AF.Exp)
                    ts("dve", sm[:, 2:3], sm[:, 1:2], 1.0, None, ALU.add)
                    recip(sm[:, 3:4], sm[:, 2:3])
                    tt("dve", sm[:, 4:5], sm[:, 1:2], sm[:, 3:4], ALU.mult)
                    ts("dve", c0, lgs, m8[:, 0:1], None, ALU.is_equal)
                    ts("dve", c0, c0, sm[:, 3:4], None, ALU.mult)
                    ts("dve", c1, lgs, m8[:, 1:2], None, ALU.is_equal)
                    ts("dve", c1, c1, sm[:, 4:5], None, ALU.mult)
                    tt("dve", comb[:, ti, :], c0, c1, ALU.add)
            nexp = 8 if moe else 1
            items = []
            for th in range(2):
                for e_ in range(nexp):
                    if moe:
                        W1 = dr["m_w1"][e_].rearrange("(c p) n -> p c n", p=128)
                        W3 = dr["m_w3"][e_].rearrange("(c p) n -> p c n", p=128)
                        W2 = dr["m_w2"][e_].rearrange("(j p) n -> p j n", p=128)
                    else:
                        W1 = dr["f_w1"][0].rearrange("(c p) n -> p c n", p=128)
                        W3 = dr["f_w3"][0].rearrange("(c p) n -> p c n", p=128)
                        W2 = dr["f_w2"][0].rearrange("(j p) n -> p j n", p=128)
                    for jp in range(NJ // 2):
                        items.append(("w13", th, e_, jp, W1, W3, W2))
                    for fq in range(4):
                        items.append(("w2", th, e_, fq, W1, W3, W2))

            def do_load(it):
                kind, th, e_, idx, W1, W3, W2 = it
                if kind == "w13":
                    wa = w13[wi[0] % 4]
                    wb = w13[(wi[0] + 1) % 4]
                    wi[0] += 2
                    wload(wa, W1[:, :, idx * 256:(idx + 1) * 256])
                    wload(wb, W3[:, :, idx * 256:(idx + 1) * 256])
                    return (wa, wb)
                else:
                    w2 = w2b[wi[1] % 2]
                    wi[1] += 1
                    wload(w2, W2[:, :, idx * 256:(idx + 1) * 256])
                    return (w2,)

            def do_compute(it, bufs):
                kind, th, e_, idx, W1, W3, W2 = it
                if kind == "w13":
                    wa, wb = bufs
                    jp = idx
                    if moe and jp == 0:
                        for t2_ in range(2):
                            tq = th * 2 + t2_
                            pc = ps_next("M")
                            for tl in range(4):
                                ti = tq * 4 + tl
                                dg = dgs[tl % 2]
                                ts("dve", dg, ident_f, comb[:, ti, e_:e_ + 1], None, ALU.mult)
                                dgh_, dgl_ = dgh[tl % 2], dgl[tl % 2]
                                tcopy("dve", dgh_, dg)
                                tt("dve", dgl_, dg, dgh_, ALU.subtract)
                                mm(pc[:, tl * 128:(tl + 1) * 128], dr_ones1, dgh_, True, False)
                                mm(pc[:, tl * 128:(tl + 1) * 128], dr_ones1, dgl_, False, True)
                            tcopy("dve", cbt[t2_], pc)
                    for jj in range(2):
                        j = jp * 2 + jj
                        for t2_ in range(2):
                            tq = th * 2 + t2_
                            pA = ps_next("O")
                            for d in range(8):
                                mm(pA, wa[:, d, jj * 128:(jj + 1) * 128], hT[:, d, cs(tq)], d == 0, d == 7)
                            pB = ps_next("O")
                            for d in range(8):
                                mm(pB, wb[:, d, jj * 128:(jj + 1) * 128], hT[:, d, cs(tq)], d == 0, d == 7)
                            s_ = sg_[(j * 2 + t2_) % 2]
                            act(s_, pA, AF.Silu)
                            if moe:
                                t_ = tf[(j * 2 + t2_) % 2]
                                tt("dve", t_, s_, pB, ALU.mult)
                                tt("pool", uT[:, j, t2_ * 512:(t2_ + 1) * 512], t_, cbt[t2_], ALU.mult)
                            else:
                                tt("dve", uT[:, j, t2_ * 512:(t2_ + 1) * 512], s_, pB, ALU.mult)
                else:
                    (w2,) = bufs
                    fq = idx
                    for f2 in range(2):
                        fo = fq * 2 + f2
                        for t2_ in range(2):
                            tq = th * 2 + t2_
                            pO = ps_next("M")
                            for j in range(NJ):
                                mm(pO, w2[:, j, f2 * 128:(f2 + 1) * 128], uT[:, j, t2_ * 512:(t2_ + 1) * 512], j == 0, j == NJ - 1)
                            tt("dve", xT[:, fo, cs(tq)], xT[:, fo, cs(tq)], pO, ALU.add)

            loaded = {}
            loaded[0] = do_load(items[0])
            for i, it in enumerate(items):
                if i + 1 < len(items):
                    loaded[i + 1] = do_load(items[i + 1])
                do_compute(it, loaded.pop(i))
            top[0] = m0

        ones1 = alloc([128, 128], BF16)
        dma("sp", ones1, dr["c_ones1_f"])
        dr_ones1 = ones1
        PH0 = top[0]

        stages = ["mixer0", "ffn0", "mixer1", "ffn1", "full"]
        si = stages.index(stop_after) if stop_after in stages else 0
        try:
            mixer(0)
        except StopBuild:
            pass
        if si >= 1:
            ffn(0)
        if si >= 2:
            mixer(1)
        if si >= 3:
            ffn(1)
        outs = []
        if si >= 4:
            sq = [alloc([128, 512], F32) for _ in range(2)]
            sqh = [alloc([128, 512], BF16) for _ in range(2)]
            sql = [alloc([128, 512], BF16) for _ in range(2)]
            rstd = [alloc([128, 512], F32) for _ in range(2)]
            for tq in range(4):
                pm = ps_next("M")
                for f in range(8):
                    s_ = sq[f % 2]
                    act(s_, xT[:, f, cs(tq)], AF.Square)
                    tcopy("dve", sqh[f % 2], s_)
                    tt("dve", sql[f % 2], s_, sqh[f % 2], ALU.subtract)
                    mm(pm, ones_f, sqh[f % 2], f == 0, False)
                    mm(pm, ones_f, sql[f % 2], False, f == 7)
                r_ = rstd[tq % 2]
                ts("dve", r_, pm, EPS, None, ALU.add)
                act(r_, r_, AF.Sqrt)
                recip(r_, r_)
                for f in range(8):
                    stt("dve", xT[:, f, cs(tq)], xT[:, f, cs(tq)], gains[:, 4, f:f + 1], r_, ALU.mult, ALU.mult)
        for f in range(8):
            o = P.op("sp", lambda e, f=f: e.dma_start(out=outT[f * 128:(f + 1) * 128, :], in_=xT.ap[:, f, :]),
                     reads=[xT[:, f, :]], dma=True)
            outs.append(o)
        fin = P.op("sp", lambda e: e.nop())
        for o in outs:
            fin.deps.add(o.id)
        P.finalize(sems, dsems, block)
    nc._declared = set(dr.keys())
    return nc, P


_CACHE = {}


def make_in_maps(inputs):
    hc = host_consts()
    x = np.asarray(inputs["x"], np.float32)
    pos = np.asarray(inputs["positions"], np.int32)
    shared = dict(hc)
    for l in range(2):
        shared["w_in%d" % l] = np.ascontiguousarray(inputs["w_in"][l], dtype=np.float32)
        shared["g_mix%d" % l] = np.ascontiguousarray(inputs["norm_mix"][l], dtype=np.float32)
        shared["g_ffn%d" % l] = np.ascontiguousarray(inputs["norm_ffn"][l], dtype=np.float32)
        shared["posk%d" % l] = np.ascontiguousarray(inputs["cmp_pos_k"][l], dtype=np.float32)
        shared["posv%d" % l] = np.ascontiguousarray(inputs["cmp_pos_v"][l], dtype=np.float32)
        shared["kw1_%d" % l] = np.ascontiguousarray(inputs["cmp_k_w1"][l], dtype=np.float32)
        shared["kw2_%d" % l] = np.ascontiguousarray(inputs["cmp_k_w2"][l], dtype=np.float32)
        shared["vw1_%d" % l] = np.ascontiguousarray(inputs["cmp_v_w1"][l], dtype=np.float32)
        shared["vw2_%d" % l] = np.ascontiguousarray(inputs["cmp_v_w2"][l], dtype=np.float32)
        shared["pa%d" % l] = np.ascontiguousarray(inputs["w_branch_a"][l], dtype=np.float32)
        shared["pb%d" % l] = np.ascontiguousarray(inputs["w_branch_b"][l], dtype=np.float32)
        shared["wo%d" % l] = np.ascontiguousarray(inputs["w_out"][l], dtype=np.float32)
    shared["f_w1"] = np.ascontiguousarray(inputs["ffn_w1"], dtype=np.float32)
    shared["f_w3"] = np.ascontiguousarray(inputs["ffn_w3"], dtype=np.float32)
    shared["f_w2"] = np.ascontiguousarray(inputs["ffn_w2"], dtype=np.float32)
    shared["router"] = np.ascontiguousarray(inputs["router"][0], dtype=np.float32)
    shared["m_w1"] = np.ascontiguousarray(inputs["moe_w1"][0], dtype=np.float32)
    shared["m_w3"] = np.ascontiguousarray(inputs["moe_w3"][0], dtype=np.float32)
    shared["m_w2"] = np.ascontiguousarray(inputs["moe_w2"][0], dtype=np.float32)
    shared["g_fin"] = np.ascontiguousarray(inputs["final_norm"], dtype=np.float32)
    maps = []
    for b in range(8):
        m = dict(shared)
        m["xT"] = np.ascontiguousarray(x[b].T)
        m["pos"] = np.ascontiguousarray(pos[b:b + 1])
        maps.append(m)
    return maps


def kernel(stop_after="full", **inputs):
    if stop_after not in _CACHE:
        _CACHE[stop_after] = build_program(stop_after)[0]
    nc = _CACHE[stop_after]
    maps = make_in_maps(inputs)
    decl = nc._declared
    maps = [{k: v for k, v in m.items() if k in decl} for m in maps]
    res = run_bass_kernel_spmd(nc, maps, core_ids=list(range(8)))
    out = np.stack([np.ascontiguousarray(r["outT"].T) for r in res.results], axis=0)
    return out.astype(np.float32)
```
